# Optimizing a Trainium2 kernel written in Bass

```python
import jax
import jax.numpy as jnp
from jax import lax
import numpy as np

D_MODEL = 2048
BATCH = 4
SEQ = 2048
DEPTH = 1
DEC_BATCH = 128
DEC_SEQ = 1
PAST_LEN = 16384
PAGE_SIZE = 128

CHUNK = 128
A_WIDTH = D_MODEL
A_GROUPS = 8
A_GROUP_DIM = A_WIDTH // A_GROUPS
B_WIDTH = D_MODEL
CONV_WIDTH = 31
MOE_GROUPS = 4
EXPERTS_PER_GROUP = 8
N_EXPERTS = MOE_GROUPS * EXPERTS_PER_GROUP
TOP_K = 2
D_EXPERT = D_MODEL // 2
MOE_BLOCK = 128
PLE_DIM = 256
EPS = 1e-6
SPLITS = [A_WIDTH, 2 * A_WIDTH, 2 * A_WIDTH + B_WIDTH, 2 * A_WIDTH + 2 * B_WIDTH,
          2 * A_WIDTH + 2 * B_WIDTH + D_MODEL]
N_IN = 2 * A_WIDTH + 2 * B_WIDTH + 2 * D_MODEL

kernel_name = 'hybrid_gmlp_conformer_hiermoe_step'


def rms_norm(x, g):
    xf = x.astype(jnp.float32)
    y = xf * lax.rsqrt(jnp.mean(xf * xf, axis=-1, keepdims=True) + EPS)
    return (y * g).astype(x.dtype)


def layer_norm(x, g, b):
    xf = x.astype(jnp.float32)
    mu = jnp.mean(xf, axis=-1, keepdims=True)
    xc = xf - mu
    y = xc * lax.rsqrt(jnp.mean(xc * xc, axis=-1, keepdims=True) + EPS)
    return (y * g + b).astype(x.dtype)


def chunk_spatial_mix(v, w_s, b_s):
    bsz, length = v.shape[0], v.shape[1]
    c = min(length, CHUNK)
    padded = -(-length // c) * c
    v = jnp.pad(v, ((0, 0), (0, padded - length), (0, 0), (0, 0)))
    v = v.reshape(bsz, padded // c, c, A_GROUPS, A_GROUP_DIM)
    w = jnp.tril(w_s[:, :c, :c])
    out = jnp.einsum('gts,bnsgd->bntgd', w, v) + b_s[:, :c].T[None, None, :, :, None]
    return out.reshape(bsz, padded, A_GROUPS, A_GROUP_DIM)[:, :length]


def hier_moe(x, w_rg, b_rg, w_re, b_re, w_gate, w_up, w_down):
    n_tok = x.shape[0]
    lg = (x @ w_rg).astype(jnp.float32) + b_rg.astype(jnp.float32)
    pg = jax.nn.softmax(lg, axis=-1)
    gi = jnp.argmax(lg, axis=-1)
    gw = jnp.take_along_axis(pg, gi[:, None], axis=-1)
    le = ((x @ w_re).astype(jnp.float32) + b_re.astype(jnp.float32)).reshape(
        n_tok, MOE_GROUPS, EXPERTS_PER_GROUP)
    le = jnp.take_along_axis(le, gi[:, None, None], axis=1)[:, 0]
    tv, ti = lax.top_k(jax.nn.softmax(le, axis=-1), TOP_K)
    ew = gw * tv / jnp.sum(tv, axis=-1, keepdims=True)
    eid = gi[:, None] * EXPERTS_PER_GROUP + ti
    n_rows = n_tok * TOP_K
    flat_e = eid.reshape(-1).astype(jnp.int32)
    flat_tok = jnp.repeat(jnp.arange(n_tok, dtype=jnp.int32), TOP_K)
    flat_w = ew.reshape(-1)
    order = jnp.argsort(flat_e)
    se, stok, sw = flat_e[order], flat_tok[order], flat_w[order]
    counts = jax.ops.segment_sum(jnp.ones_like(flat_e), flat_e, num_segments=N_EXPERTS)
    starts = jnp.cumsum(counts) - counts
    pcounts = (counts + MOE_BLOCK - 1) // MOE_BLOCK * MOE_BLOCK
    pends = jnp.cumsum(pcounts)
    pstarts = pends - pcounts
    dest = pstarts[se] + jnp.arange(n_rows, dtype=jnp.int32) - starts[se]
    n_blocks = -(-n_rows // MOE_BLOCK) + N_EXPERTS
    buf_tok = jnp.full((n_blocks * MOE_BLOCK,), n_tok, jnp.int32).at[dest].set(stok)
    buf_w = jnp.zeros((n_blocks * MOE_BLOCK,), jnp.float32).at[dest].set(sw)
    blk_e = jnp.minimum(jnp.searchsorted(pends, jnp.arange(n_blocks, dtype=jnp.int32) * MOE_BLOCK,
                                         side='right'), N_EXPERTS - 1)
    x_pad = jnp.concatenate([x, jnp.zeros((1, x.shape[1]), x.dtype)], axis=0)
    xb = x_pad[buf_tok].reshape(n_blocks, MOE_BLOCK, x.shape[1])

    def expert_block(args):
        xe, e = args
        h = jax.nn.silu(xe @ w_gate[e]) * (xe @ w_up[e])
        return h @ w_down[e]

    yb = lax.map(expert_block, (xb, blk_e)).reshape(n_blocks * MOE_BLOCK, x.shape[1])
    yb = (yb * buf_w[:, None]).astype(x.dtype)
    return jax.ops.segment_sum(yb, buf_tok, num_segments=n_tok + 1)[:n_tok]


def trunk_layer(x, p, conv_prefix, norm_mix, w_in, ln_v_g, ln_v_b, w_spatial, b_spatial, w_proj_a,
                conv_w, conv_b, ln_c_g, ln_c_b, w_proj_b, w_out, norm_ffn, w_rg, b_rg, w_re, b_re,
                w_eg, w_eu, w_ed, norm_ple, w_ple_gate, w_ple_proj):
    bsz, length, _ = x.shape
    xn = rms_norm(x, norm_mix)
    z = xn @ w_in
    u, v, c_val, c_gate, g_a, g_b = jnp.split(z, SPLITS, axis=-1)
    u = jax.nn.gelu(u)
    v = layer_norm(jax.nn.gelu(v), ln_v_g, ln_v_b)
    s = chunk_spatial_mix(v.reshape(bsz, length, A_GROUPS, A_GROUP_DIM), w_spatial, b_spatial)
    branch_a = (u * s.reshape(bsz, length, A_WIDTH)) @ w_proj_a
    glu = c_val * jax.nn.sigmoid(c_gate)
    conv_in = jnp.concatenate([conv_prefix, glu], axis=1)
    conv = lax.conv_general_dilated(conv_in, conv_w[:, None, :], window_strides=(1,), padding='VALID',
                                    dimension_numbers=('NWC', 'WIO', 'NWC'),
                                    feature_group_count=B_WIDTH) + conv_b
    branch_b = jax.nn.silu(layer_norm(conv, ln_c_g, ln_c_b)) @ w_proj_b
    h = x + (jax.nn.sigmoid(g_a) * branch_a + jax.nn.sigmoid(g_b) * branch_b) @ w_out
    hn = rms_norm(h, norm_ffn).reshape(bsz * length, D_MODEL)
    h = h + hier_moe(hn, w_rg, b_rg, w_re, b_re, w_eg, w_eu, w_ed).reshape(bsz, length, D_MODEL)
    h = h + jax.nn.sigmoid(rms_norm(h, norm_ple) @ w_ple_gate) * (p @ w_ple_proj)
    return h, conv_in[:, -(CONV_WIDTH - 1):], v


def setup_inputs(seed: int = 0) -> dict:
    key = jax.random.key(seed)
    ks = iter(jax.random.split(key, 40))

    def nrm(shape, scale):
        return jax.random.normal(next(ks), shape, jnp.float32) * scale

    def gain(shape):
        return 1.0 + nrm(shape, 0.02)

    return {
        'x_prompt': nrm((BATCH, SEQ, D_MODEL), 1.0),
        'x_sample': nrm((DEC_BATCH, DEC_SEQ, D_MODEL), 1.0),
        'state_conv': nrm((DEPTH, DEC_BATCH, CONV_WIDTH - 1, B_WIDTH), 0.5),
        'p_prompt': nrm((DEPTH, BATCH, SEQ, PLE_DIM), 1.0),
        'p_sample': nrm((DEPTH, DEC_BATCH, DEC_SEQ, PLE_DIM), 1.0),
        'norm_mix': gain((DEPTH, D_MODEL)),
        'w_in': nrm((DEPTH, D_MODEL, N_IN), D_MODEL ** -0.5),
        'ln_v_g': gain((DEPTH, A_WIDTH)),
        'ln_v_b': nrm((DEPTH, A_WIDTH), 0.02),
        'w_spatial': nrm((DEPTH, A_GROUPS, CHUNK, CHUNK), CHUNK ** -0.5),
        'b_spatial': 1.0 + nrm((DEPTH, A_GROUPS, CHUNK), 0.1),
        'w_proj_a': nrm((DEPTH, A_WIDTH, D_MODEL), A_WIDTH ** -0.5),
        'conv_w': nrm((DEPTH, CONV_WIDTH, B_WIDTH), CONV_WIDTH ** -0.5),
        'conv_b': nrm((DEPTH, B_WIDTH), 0.02),
        'ln_c_g': gain((DEPTH, B_WIDTH)),
        'ln_c_b': nrm((DEPTH, B_WIDTH), 0.02),
        'w_proj_b': nrm((DEPTH, B_WIDTH, D_MODEL), B_WIDTH ** -0.5),
        'w_out': nrm((DEPTH, D_MODEL, D_MODEL), D_MODEL ** -0.5),
        'norm_ffn': gain((DEPTH, D_MODEL)),
        'w_router_group': nrm((DEPTH, D_MODEL, MOE_GROUPS), D_MODEL ** -0.5),
        'b_router_group': nrm((DEPTH, MOE_GROUPS), 0.01),
        'w_router_expert': nrm((DEPTH, D_MODEL, N_EXPERTS), D_MODEL ** -0.5),
        'b_router_expert': nrm((DEPTH, N_EXPERTS), 0.01),
        'w_exp_gate': nrm((DEPTH, N_EXPERTS, D_MODEL, D_EXPERT), D_MODEL ** -0.5),
        'w_exp_up': nrm((DEPTH, N_EXPERTS, D_MODEL, D_EXPERT), D_MODEL ** -0.5),
        'w_exp_down': nrm((DEPTH, N_EXPERTS, D_EXPERT, D_MODEL), D_EXPERT ** -0.5),
        'norm_ple': gain((DEPTH, D_MODEL)),
        'w_ple_gate': nrm((DEPTH, D_MODEL, D_MODEL), D_MODEL ** -0.5),
        'w_ple_proj': nrm((DEPTH, PLE_DIM, D_MODEL), PLE_DIM ** -0.5),
        'final_norm': gain((D_MODEL,)),
    }


def reference(x_prompt, x_sample, state_conv, p_prompt, p_sample, norm_mix, w_in, ln_v_g, ln_v_b,
              w_spatial, b_spatial, w_proj_a, conv_w, conv_b, ln_c_g, ln_c_b, w_proj_b, w_out,
              norm_ffn, w_router_group, b_router_group, w_router_expert, b_router_expert,
              w_exp_gate, w_exp_up, w_exp_down, norm_ple, w_ple_gate, w_ple_proj, final_norm):
    h_p, h_s = x_prompt, x_sample
    conv_p, conv_s, chunk_v_s = [], [], []
    for i in range(DEPTH):
        lp = (norm_mix[i], w_in[i], ln_v_g[i], ln_v_b[i], w_spatial[i], b_spatial[i], w_proj_a[i],
              conv_w[i], conv_b[i], ln_c_g[i], ln_c_b[i], w_proj_b[i], w_out[i], norm_ffn[i],
              w_router_group[i], b_router_group[i], w_router_expert[i], b_router_expert[i],
              w_exp_gate[i], w_exp_up[i], w_exp_down[i], norm_ple[i], w_ple_gate[i], w_ple_proj[i])
        prefix = jnp.zeros((h_p.shape[0], CONV_WIDTH - 1, B_WIDTH), h_p.dtype)
        h_p, cp, _ = trunk_layer(h_p, p_prompt[i], prefix, *lp)
        h_s, cs, vs = trunk_layer(h_s, p_sample[i], state_conv[i], *lp)
        conv_p.append(cp)
        conv_s.append(cs)
        chunk_v_s.append(vs)
    y_prompt = rms_norm(h_p, final_norm)
    y_sample = rms_norm(h_s, final_norm)
    return (y_prompt, y_sample, jnp.stack(conv_p), jnp.stack(conv_s), jnp.stack(chunk_v_s))
```

```python
import contextlib
import numpy as np
import concourse.bass as bass
import concourse.mybir as mybir
from concourse.bass_utils import run_bass_kernel_spmd

F32 = mybir.dt.float32
BF16 = mybir.dt.bfloat16
U32 = mybir.dt.uint32
AF = mybir.ActivationFunctionType
ALU = mybir.AluOpType
AX = mybir.AxisListType

NCORES = 8
D = 2048
KT = 16
NPC = 1024
NSC = 16
NTOK = NPC + NSC
NE = 32
DE = 1024
CAP = 128
NSLOT = NE * CAP
EPS = 1e-6
NL = 48
RING = 3
CH = 8192
DEBUG = False


def semkey(s):
    return getattr(s, "num", None) if getattr(s, "num", None) is not None else id(s)


class Ctx:
    def __init__(self, nc, es):
        self.nc = nc
        self.eng = dict(pe=nc.tensor, act=nc.scalar, dve=nc.vector, pool=nc.gpsimd, sp=nc.sync)
        self.csem = {e: es.enter_context(nc.semaphore("s_" + e)) for e in self.eng}
        self.ccnt = {e: 0 for e in self.eng}
        self.waited = {e: {} for e in self.eng}
        self.lanes = [es.enter_context(nc.semaphore("l%d" % i)) for i in range(NL)]
        self.lane_cnt = [0] * NL
        self.next_lane = 0
        self.lw = {}
        self.rd = {}
        self.t = {"pe": 0.0, "dve": 0.0}
        self.sems = {}
        for s in list(self.csem.values()) + self.lanes:
            self.sems[id(s)] = s

    def _wait(self, e, sid, val):
        if self.waited[e].get(sid, 0) >= val:
            return
        self.eng[e].wait_ge(self.sems[sid], val)
        self.waited[e][sid] = val

    def global_barrier(self):
        m = {}
        for en, s in self.csem.items():
            if self.ccnt[en]:
                m[id(s)] = self.ccnt[en]
        for i, s in enumerate(self.lanes):
            if self.lane_cnt[i]:
                m[id(s)] = self.lane_cnt[i]
        self.floor = {en: dict(m) for en in self.eng}

    def deps(self, e, reads, writes):
        fl = getattr(self, "floor", {}).get(e)
        if fl:
            for sid, v in fl.items():
                self._wait(e, sid, v)
            self.floor[e] = None
        need = {}
        for r in reads:
            for sid, v in self.lw.get(r, {}).items():
                need[sid] = max(need.get(sid, 0), v)
        for w in writes:
            for sid, v in self.lw.get(w, {}).items():
                need[sid] = max(need.get(sid, 0), v)
            for sid, v in self.rd.get(w, {}).items():
                need[sid] = max(need.get(sid, 0), v)
        for sid, v in need.items():
            self._wait(e, sid, v)

    def done(self, sid, val, reads, writes):
        for w in writes:
            self.lw[w] = {sid: val}
            self.rd[w] = {}
        for r in reads:
            if r in writes:
                continue
            d = self.rd.setdefault(r, {})
            d[sid] = max(d.get(sid, 0), val)

    def alias(self, new_keys, old_keys):
        m = {}
        for k in old_keys:
            for sid, v in self.lw.get(k, {}).items():
                m[sid] = max(m.get(sid, 0), v)
            for sid, v in self.rd.get(k, {}).items():
                m[sid] = max(m.get(sid, 0), v)
        for k in new_keys:
            cur = dict(self.lw.get(k, {}))
            for sid, v in m.items():
                cur[sid] = max(cur.get(sid, 0), v)
            self.lw[k] = cur

    def barrier_keys(self, new_keys):
        m = {}
        for e, s in self.csem.items():
            if self.ccnt[e]:
                m[id(s)] = self.ccnt[e]
        for i, s in enumerate(self.lanes):
            if self.lane_cnt[i]:
                m[id(s)] = self.lane_cnt[i]
        for k in new_keys:
            self.lw[k] = dict(m)
            self.rd[k] = {}

    def op(self, e, fn, reads=(), writes=(), cost=0.65):
        if e == "dve":
            self.t["dve"] += cost
        self.deps(e, reads, writes)
        inst = fn(self.eng[e])
        self.ccnt[e] += 1
        inst.then_inc(self.csem[e], 1)
        self.done(id(self.csem[e]), self.ccnt[e], reads, writes)

    def mm(self, fns, reads=(), writes=(), cost=None):
        self.t["pe"] += (0.215 * len(fns)) if cost is None else cost
        self.deps("pe", reads, writes)
        inst = None
        for fn in fns:
            inst = fn(self.eng["pe"])
        self.ccnt["pe"] += 1
        inst.then_inc(self.csem["pe"], 1)
        self.done(id(self.csem["pe"]), self.ccnt["pe"], reads, writes)

    def dma(self, e, fn, reads=(), writes=()):
        self.deps(e, reads, writes)
        L = self.next_lane
        self.next_lane = (L + 1) % NL
        if self.lane_cnt[L] > 0:
            self._wait(e, id(self.lanes[L]), self.lane_cnt[L])
        inst = fn(self.eng[e])
        self.lane_cnt[L] += 16
        inst.then_inc(self.lanes[L], 16)
        self.done(id(self.lanes[L]), self.lane_cnt[L], reads, writes)

    def final_wait(self):
        e = "sp"
        for i, s in enumerate(self.lanes):
            if self.lane_cnt[i]:
                self._wait(e, id(s), self.lane_cnt[i])
        for en, s in self.csem.items():
            if en != e and self.ccnt[en]:
                self._wait(e, id(s), self.ccnt[en])


def build_program():
    nc = bass.Bass("TRN2", target_bir_lowering=False)

    def din(name, shape, dt=F32):
        return nc.dram_tensor(name, list(shape), dt, kind="ExternalInput").ap()

    def dout(name, shape, dt=F32):
        return nc.dram_tensor(name, list(shape), dt, kind="ExternalOutput").ap()

    xT = din("xT", [D, NTOK])
    xh = din("xh", [D, 32])
    xtok = din("xtok", [NTOK, D])
    pT = din("pT", [256, NTOK])
    stT = din("stT", [D, NSC * 30])
    st = din("st", [NSC, 30 * D])
    w_in = din("w_in", [D, 6 * D])
    w_pa = din("w_pa", [D, D])
    w_pb = din("w_pb", [D, D])
    w_out = din("w_out", [D, D])
    w_pg = din("w_pg", [D, D])
    w_pp = din("w_pp", [256, D])
    w_eg = din("w_eg", [NE, D, DE])
    w_eu = din("w_eu", [NE, D, DE])
    w_ed = din("w_ed", [NE, DE, D])
    wr = din("wr", [D, 36])
    br = din("br", [1, 36])
    colv = din("colv", [128, 9 * KT])
    colv2 = din("colv2", [128, KT])
    cw = din("cw", [128, KT * 31])
    wsT = din("wsT", [128, 8 * 128])
    bs = din("bs", [1, 8 * 128])
    gfin = din("gfin", [1, D])
    cst = din("cst", [128, 128 * 3 + 32 + 1 + 9])

    y = dout("y", [NTOK, D])
    ncp = dout("ncp", [30, D])
    ncs = dout("ncs", [NSC, 30 * D])
    ncv = dout("ncv", [NSC, D])
    dbg = {}
    if DEBUG:
        dbg["d_xn"] = dout("d_xn", [128, KT * 560], BF16)
        dbg["d_glu"] = dout("d_glu", [128, KT * 560], BF16)
        dbg["d_v"] = dout("d_v", [128, 4 * D], BF16)
        dbg["d_us"] = dout("d_us", [128, KT * 528], BF16)
        dbg["d_cb"] = dout("d_cb", [128, KT * 560], BF16)
        dbg["d_mg"] = dout("d_mg", [128, KT * 528], BF16)
        dbg["d_lg"] = dout("d_lg", [128, 9 * 36])
        dbg["d_rt"] = dout("d_rt", [128, 9 * 8])

    hscr = nc.dram_tensor("hscr", [9 * 128, D], F32).ap()
    hnscr = nc.dram_tensor("hnscr", [9 * 128, D], BF16).ap()
    xg = nc.dram_tensor("xg", [NSLOT + 128, D], BF16).ap()
    yscr = nc.dram_tensor("yscr", [NSLOT + 128, D], F32).ap()

    es = contextlib.ExitStack()
    with es:
        cx = Ctx(nc, es)

        def sb(name, shape, dt=F32, stack=es):
            return stack.enter_context(nc.sbuf_tensor(name, list(shape), dt))

        psb = [es.enter_context(nc.psum_tensor("ps%d" % i, [128, 512], F32)) for i in range(8)]
        pstate = {"i": 0}

        def nextps():
            i = pstate["i"]
            pstate["i"] = (i + 1) % 8
            return i

        cst_t = sb("cst_t", [128, 128 * 3 + 32 + 1 + 9])
        colv_t = sb("colv_t", [128, 9 * KT])
        colv2_t = sb("colv2_t", [128, KT])
        cw_t = sb("cw_t", [128, KT, 31])
        eps_c = sb("eps_c", [128, 1])
        ident_b = sb("ident_b", [128, 128], BF16)
        ones_b = sb("ones_b", [128, 128], BF16)
        triU_b = sb("triU_b", [128, 128], BF16)
        odiv = sb("odiv", [128, 128])
        ring = sb("ring", [128, RING, CH], BF16)
        posu = sb("posu", [128, 9, 2], U32)
        eww = sb("eww", [128, 9, 2])
        lg = sb("lg", [128, 9, 36])
        rstd_h = sb("rstd_h", [128, 9])

        ident_f = cst_t[:, 0:128]
        maskT = cst_t[:, 128:256]
        triU_f = cst_t[:, 256:384]
        base_e = cst_t[:, 384:416]
        trash_c = cst_t[:, 416:417]
        tokvalid = cst_t[:, 417:426]

        def col(i, k, M=128):
            return colv_t[0:M, i * KT + k:i * KT + k + 1]
        GM, LVG, LVB, CVB, LCG, LCB, WSD, BS0, GF = range(9)

        cx.dma("sp", lambda e: e.dma_start(out=cst_t[:], in_=cst[:, :]), writes=["cst"])
        cx.dma("sp", lambda e: e.dma_start(out=colv_t[:], in_=colv[:, :]), writes=["colv"])
        cx.dma("sp", lambda e: e.dma_start(out=colv2_t[:], in_=colv2[:, :]), writes=["colv2"])
        cx.dma("sp", lambda e: e.dma_start(out=cw_t[:].rearrange("p k j -> p (k j)"), in_=cw[:, :]), writes=["cw"])
        cx.op("dve", lambda e: e.memset(eps_c[:], EPS), writes=["eps"])
        cx.op("dve", lambda e: e.memset(ones_b[:], 1.0), writes=["ones_b"])
        cx.op("dve", lambda e: e.memset(odiv[:], 1.0 / D), writes=["odiv"])
        cx.op("dve", lambda e: e.tensor_copy(out=ident_b[:], in_=ident_f), reads=["cst"], writes=["ident_b"])
        cx.op("dve", lambda e: e.tensor_copy(out=triU_b[:], in_=triU_f), reads=["cst"], writes=["triU_b"])


        chunks = []

        def wsrc(w2d, c0, ncols, kt):
            return w2d.rearrange("(k p) n -> p k n", p=128)[:, :, c0:c0 + ncols]

        wstate = {"next": 0}

        extra = {}
        slots = []

        def chunk_view(j):
            srcap, a, b = chunks[j]
            kind, slot = slots[j]
            if kind == "xn":
                v = extra["xn"][:].rearrange("p k n -> p (k n)")[:, 0:a * b].rearrange("p (k n) -> p k n", k=a)
                return v, [("xn", k) for k in range(KT)]
            if slot < RING:
                base = ring[:, slot, 0:a * b]
            else:
                base = extra["ring2"][:, slot - RING, 0:a * b]
            return base.rearrange("p (k n) -> p k n", k=a), [("ring", slot)]

        pool_tok = []
        MAXFLY = 3

        def pool_dma(fn, reads=(), writes=()):
            if len(pool_tok) >= MAXFLY:
                sid, val = pool_tok[-MAXFLY]
                cx._wait("pool", sid, val)
            cx.dma("pool", fn, reads=list(reads), writes=list(writes))
            L = (cx.next_lane - 1) % NL
            pool_tok.append((id(cx.lanes[L]), cx.lane_cnt[L]))

        def emit_wdma(j):
            srcap = chunks[j][0]
            dst, keys = chunk_view(j)
            pool_dma(lambda e: e.dma_start(out=dst, in_=srcap), writes=keys)

        released = set()
        preds = {}

        def allowed(n):
            if n >= phase2_start[0] and "ring2" not in extra and slots[n][1] >= RING:
                return False
            p = preds.get(n)
            return p is None or p in released

        def prefetch():
            while wstate["next"] < len(chunks) and allowed(wstate["next"]):
                emit_wdma(wstate["next"])
                wstate["next"] += 1

        def use_chunk(j):
            prefetch()
            assert j < wstate["next"], ("chunk not loadable", j, wstate["next"])
            v, keys = chunk_view(j)
            return v, keys

        def release(*js):
            for j in js:
                released.add(j)
            prefetch()

        sched = {}
        for g in range(2):
            for c in range(4):
                sched[("gate", g, c)] = len(chunks); chunks.append((wsrc(w_in, 3 * D + c * 512, 512, KT), KT, 512))
                sched[("val", g, c)] = len(chunks); chunks.append((wsrc(w_in, 2 * D + c * 512, 512, KT), KT, 512))
            for c in range(4):
                sched[("v", g, c)] = len(chunks); chunks.append((wsrc(w_in, D + c * 512, 512, KT), KT, 512))
            for c in range(4):
                sched[("u", g, c)] = len(chunks); chunks.append((wsrc(w_in, c * 512, 512, KT), KT, 512))
            for c in range(4):
                sched[("ga", g, c)] = len(chunks); chunks.append((wsrc(w_in, 4 * D + c * 512, 512, KT), KT, 512))
                sched[("pa", g, c)] = len(chunks); chunks.append((wsrc(w_pa, c * 512, 512, KT), KT, 512))
            for c in range(4):
                sched[("gb", g, c)] = len(chunks); chunks.append((wsrc(w_in, 5 * D + c * 512, 512, KT), KT, 512))
                sched[("pb", g, c)] = len(chunks); chunks.append((wsrc(w_pb, c * 512, 512, KT), KT, 512))
            for c in range(4):
                sched[("wo", g, c)] = len(chunks); chunks.append((wsrc(w_out, c * 512, 512, KT), KT, 512))
        for ex in range(NE):
            for c in range(2):
                sched[("eg", ex, c)] = len(chunks); chunks.append((wsrc(w_eg[ex], c * 512, 512, KT), KT, 512))
                sched[("eu", ex, c)] = len(chunks); chunks.append((wsrc(w_eu[ex], c * 512, 512, KT), KT, 512))
            for c in range(2):
                sched[("ed", ex, c)] = len(chunks); chunks.append((wsrc(w_ed[ex], c * 1024, 1024, 8), 8, 1024))

        phase2_start = [sched[("eg", 0, 0)]]
        ctr = 0
        for j in range(len(chunks)):
            if j < phase2_start[0]:
                isxn = any(j == sched[("wo", g, 3)] for g in range(2))
                if isxn:
                    slots.append(("xn", -1))
                else:
                    slots.append(("ring", ctr % RING)); ctr += 1
            else:
                slots.append(("ring", (j - phase2_start[0]) % (2 * RING)))
        lastown = {}
        for j in range(len(chunks)):
            if slots[j] in lastown:
                preds[j] = lastown[slots[j]]
            lastown[slots[j]] = j

        ms = contextlib.ExitStack()
        with ms:
            A = sb("A", [128, KT * 528], BF16, ms)
            xn = sb("xn", [128, KT, 560], BF16, ms)
            glu = sb("glu", [128, KT, 560], BF16, ms)
            xs = [sb("xs%d" % i, [128, 560], F32, ms) for i in range(2)]
            usT = sb("usT", [128, KT, 528], BF16, ms)
            sigbuf = sb("sigbuf", [128, 4, 560], F32, ms)
            carry = sb("carry", [128, KT, 32], BF16, ms)
            glu_sl = sb("glu_sl", [128, KT, 32], F32, ms)
            glu_s = glu_sl[:, :, 0:16]
            glu_last = glu_sl
            rstd_bc = sb("rstd_bc", [128, 560], F32, ms)
            sqt = [sb("sqt%d" % i, [128, 560], BF16, ms) for i in range(2)]
            cs16 = sb("cs16", [128, 16], F32, ms)
            dg = [sb("dg%d" % i, [128, 31, 128], BF16, ms) for i in range(2)]
            csq_t = sb("csq_t", [128, 528], F32, ms)
            csum = sb("csum", [128, 528], F32, ms)
            csq = sb("csq", [128, 528], F32, ms)
            mean_bc = sb("mean_bc", [128, 528], F32, ms)
            rstdc = sb("rstdc", [128, 528], F32, ms)
            tt = [sb("tt%d" % i, [128, 528], F32, ms) for i in range(3)]
            trilWT = sb("trilWT", [128, 8, 128], BF16, ms)
            Cm = sb("Cm", [128, KT, 128], BF16, ms)
            vst = sb("vst", [128, 5, 4, 6], F32, ms)
            mv = sb("mv", [128, 5, 2], F32, ms)
            vrs = sb("vrs", [128, 5], F32, ms)
            vsT = sb("vsT", [128, KT, 16], F32, ms)
            ssT = sb("ssT", [128, KT, 16], F32, ms)
            stc = [sb("stc0", [128, 16, 30], F32, ms)] * 2
            stm = sb("stm", [128, 16, 30], F32, ms)
            accs = sb("accs", [128, 16], F32, ms)
            xtc = [sb("xtc%d" % i, [128, 512], F32, ms) for i in range(2)]
            hub = [sb("hub0", [128, D], BF16, ms)] * 2
            hT = sb("hT", [128, KT, 128], F32, ms)
            hTf = hT[:].rearrange("p k t -> p (k t)")
            vs_f = hTf[0:NSC, :]
            osb = hTf[0:32, :]
            wsT_t = hT[:, 0:8, :]
            bs_bc = hT[:, 8:16, :]
            wrg = sb("wrg", [128, KT, 36], F32, ms)
            br_bc = sb("br_bc", [128, 36], F32, ms)
            ssq = sb("ssq", [128, 2], F32, ms)

            extra["xn"] = xn
            A_ma = A[:].rearrange("p (k n) -> p k n", k=KT)
            A_v = A[:, 0:4 * D].rearrange("p (t n) -> p t n", t=4)
            A_h = A[:, 0:4 * D].bitcast(F32).rearrange("p (t n) -> p t n", t=2)

            cx.dma("sp", lambda e: e.dma_start(out=hTf[:, 0:1024], in_=wsT[:, :]), writes=["hTb"])
            cx.dma("sp", lambda e: e.dma_start(out=hTf[:, 1024:2048], in_=bs[0:1, :].partition_broadcast(128)), reads=["hTb"], writes=["hTb"])
            cx.dma("sp", lambda e: e.dma_start(out=br_bc[:], in_=br[0:1, :].partition_broadcast(128)), writes=["br_bc"])
            cx.dma("sp", lambda e: e.dma_start(out=wrg[:], in_=wr.rearrange("(k p) n -> p k n", p=128)), writes=["wrg"])
            for gi in range(8):
                cx.op("dve", lambda e, gi=gi: e.tensor_tensor(out=trilWT[:, gi, :], in0=wsT_t[:, gi, :], in1=maskT, op=ALU.mult),
                      reads=["hTb", "cst"], writes=[("tril", gi)])
            for k in range(KT):
                cx.op("dve", lambda e, k=k: e.tensor_scalar(out=wrg[:, k, :], in0=wrg[:, k, :], scalar1=col(GF, k), scalar2=None, op0=ALU.mult),
                      reads=["wrg", "colv"], writes=["wrg"])
            for half in range(2):
                pi = nextps()
                cx.mm([lambda e, gi=gi, pi=pi: e.matmul(psb[pi][:, (gi % 4) * 128:(gi % 4 + 1) * 128], lhsT=ones_b[:], rhs=trilWT[:, gi, :], start=True, stop=True)
                       for gi in range(half * 4, half * 4 + 4)],
                      reads=["ones_b"] + [("tril", gi) for gi in range(half * 4, half * 4 + 4)], writes=[("ps", pi)])
                for ft in range(half * 8, half * 8 + 8):
                    gq = (ft // 2) % 4
                    cx.op("dve", lambda e, ft=ft, gq=gq, pi=pi: e.scalar_tensor_tensor(out=Cm[:, ft, :], in0=psb[pi][:, gq * 128:(gq + 1) * 128], scalar=col(LVB, ft), in1=bs_bc[:, ft // 2, :], op0=ALU.mult, op1=ALU.add),
                          reads=[("ps", pi), "hTb", "colv"], writes=[("Cm", ft)])

            for g in range(2):
                has_s = (g == 0)
                c0 = g * 512
                NTg = 528 if has_s else 512
                NXg = 560 if has_s else 512
                mt = [(0, 512)] + ([(512, 16)] if has_s else [])
                mt_vg = [(0, 512)] + ([(512, 48)] if has_s else [])
                ntile = 5 if has_s else 4

                def tile_cols(t):
                    return (t * 128, 128) if t < 4 else (512, 16)

                xTv = xT.rearrange("(k p) n -> p k n", p=128)
                xhv = xh.rearrange("(k p) n -> p k n", p=128)

                def load_x(k):
                    xb_ = xs[k % 2]
                    kk = ("xs", k % 2)
                    cx.dma("sp", lambda e: e.dma_start(out=xb_[:, 0:512], in_=xTv[:, k, c0:c0 + 512]), writes=[kk])
                    if has_s:
                        cx.dma("sp", lambda e: e.dma_start(out=xb_[:, 512:528], in_=xTv[:, k, NPC:NPC + 16]), reads=[kk], writes=[kk])
                        cx.dma("sp", lambda e: e.dma_start(out=xb_[:, 528:560], in_=xhv[:, k, :]), reads=[kk], writes=[kk])
                    return xb_, kk
                pa_ = nextps()
                pb_ = nextps() if has_s else None
                for k in range(KT):
                    xb_, kk = load_x(k)
                    cx.op("act", lambda e, k=k, xb_=xb_: e.activation(out=sqt[k % 2][:, 0:NXg], in_=xb_[:, 0:NXg], func=AF.Square),
                          reads=[kk], writes=[("sqt", k % 2)])
                    fns = [lambda e, k=k: e.matmul(psb[pa_][:, :], lhsT=ones_b[:], rhs=sqt[k % 2][:, 0:512], start=(k == 0), stop=(k == KT - 1))]
                    wr_ = [("ps", pa_)]
                    if has_s:
                        fns.append(lambda e, k=k: e.matmul(psb[pb_][:, 0:48], lhsT=ones_b[:], rhs=sqt[k % 2][:, 512:560], start=(k == 0), stop=(k == KT - 1)))
                        wr_.append(("ps", pb_))
                    cx.mm(fns, reads=[("sqt", k % 2), "ones_b"], writes=wr_)
                cx.op("act", lambda e: e.activation(out=rstd_bc[:, 0:512], in_=psb[pa_][:, :], func=AF.Sqrt, bias=eps_c[:], scale=1.0 / D),
                      reads=[("ps", pa_), "eps"], writes=["rstd_bc"])
                if has_s:
                    cx.op("act", lambda e: e.activation(out=rstd_bc[:, 512:560], in_=psb[pb_][:, 0:48], func=AF.Sqrt, bias=eps_c[:], scale=1.0 / D),
                          reads=[("ps", pb_), "eps", "rstd_bc"], writes=["rstd_bc"])
                cx.op("dve", lambda e: e.reciprocal(out=rstd_bc[:, 0:NXg], in_=rstd_bc[:, 0:NXg]), reads=["rstd_bc"], writes=["rstd_bc"])
                for k in range(KT):
                    xb_, kk = load_x(k)
                    cx.op("dve", lambda e, k=k, xb_=xb_: e.scalar_tensor_tensor(out=xn[:, k, 0:NXg], in0=xb_[:, 0:NXg], scalar=col(GM, k), in1=rstd_bc[:, 0:NXg], op0=ALU.mult, op1=ALU.mult),
                          reads=[kk, "rstd_bc", "colv"], writes=[("xn", k)])
                xnk = [("xn", k) for k in range(KT)]
                if DEBUG and g == 0:
                    cx.dma("sp", lambda e: e.dma_start(out=dbg["d_xn"][:, :], in_=xn[:].rearrange("p k n -> p (k n)")), reads=xnk)
                vkeys = [("vf", t) for t in range(4)]
                cx.alias(vkeys, ["A_h", ("h", 0), ("h", 1)])
                if g == 1:
                    for ft in range(KT):
                        cx.op("act", lambda e, ft=ft: e.activation(out=glu[:, ft, 0:32], in_=carry[:, ft, :], func=AF.Copy),
                              reads=[("carry", ft)], writes=[("glu", ft)])

                pend = []
                built = []
                cst8 = {"tick": 0}
                cstate = {"done": True}

                def conv_fill(slack=0.0):
                    return

                def emit_build(ct):
                    d = dg[ct % 2]
                    cx.op("dve", lambda e: e.tensor_tensor(out=d[:, :, :],
                                                           in0=ident_b[:].rearrange("p (o n) -> p o n", o=1).to_broadcast([128, 31, 128]),
                                                           in1=cw_t[:, ct, :].rearrange("p (j o) -> p j o", o=1).to_broadcast([128, 31, 128]), op=ALU.mult),
                          reads=["ident_b", "cw"], writes=[("dg", ct % 2)], cost=4.2)

                def emit_conv(ct):
                    d = dg[ct % 2]
                    pi = nextps()
                    cx.mm([lambda e, j=j: e.matmul(psb[pi][:, :], lhsT=d[:, j, :], rhs=glu[:, ct, 2 + j:514 + j], start=(j == 0), stop=(j == 30)) for j in range(31)],
                          reads=[("dg", ct % 2), ("glu", ct)], writes=[("ps", pi)])
                    T1 = xs[ct % 2]
                    tk1 = ("xs", ct % 2)
                    cx.op("act", lambda e: e.activation(out=T1[:, 0:512], in_=psb[pi][:, :], func=AF.Identity, bias=col(CVB, ct), scale=1.0),
                          reads=[("ps", pi), "colv"], writes=[tk1])
                    cx.op("act", lambda e: e.activation(out=csq_t[:, 0:512], in_=psb[pi][:, :], func=AF.Square, bias=col(CVB, ct), scale=1.0),
                          reads=[("ps", pi), "colv", "csq_t"], writes=["csq_t"])
                    cx.op("dve", lambda e: e.tensor_copy(out=glu[:, ct, 32:544], in_=T1[:, 0:512]), reads=[tk1, ("glu", ct)], writes=[("glu", ct)])
                    if ct == 0:
                        cx.op("dve", lambda e: e.tensor_copy(out=csum[:, 0:512], in_=T1[:, 0:512]), reads=[tk1], writes=["csum"])
                        cx.op("dve", lambda e: e.tensor_copy(out=csq[:, 0:512], in_=csq_t[:, 0:512]), reads=["csq_t"], writes=["csq"])
                    else:
                        cx.op("dve", lambda e: e.tensor_tensor(out=csum[:, 0:512], in0=csum[:, 0:512], in1=T1[:, 0:512], op=ALU.add), reads=[tk1, "csum"], writes=["csum"])
                        cx.op("dve", lambda e: e.tensor_tensor(out=csq[:, 0:512], in0=csq[:, 0:512], in1=csq_t[:, 0:512], op=ALU.add), reads=["csq_t", "csq"], writes=["csq"])
                    if has_s:
                        sc = stc[0]
                        sk = ("stc", 0)
                        cx.dma("sp", lambda e: e.dma_start(out=sc[:].rearrange("p b j -> p (b j)"), in_=stT[ct * 128:(ct + 1) * 128, :]), writes=[sk])
                        cx.op("dve", lambda e: e.tensor_tensor(out=stm[:], in0=sc[:], in1=cw_t[:, ct:ct + 1, 0:30].to_broadcast([128, 16, 30]), op=ALU.mult),
                              reads=[sk, "cw"], writes=["stm"])
                        cx.op("dve", lambda e: e.tensor_reduce(out=accs[:], in_=stm[:], axis=AX.X, op=ALU.add), reads=["stm"], writes=["accs"])
                        cx.op("dve", lambda e: e.scalar_tensor_tensor(out=accs[:], in0=glu_s[:, ct, :], scalar=cw_t[:, ct, 30:31], in1=accs[:], op0=ALU.mult, op1=ALU.add),
                              reads=[("glu_s", ct), "accs", "cw"], writes=["accs"])
                        cx.op("dve", lambda e: e.tensor_scalar(out=cs16[:], in0=accs[:], scalar1=col(CVB, ct), scalar2=None, op0=ALU.add), reads=["accs", "colv", "cs16"], writes=["cs16"])
                        cx.op("act", lambda e: e.activation(out=glu[:, ct, 544:560], in_=cs16[:], func=AF.Copy), reads=["cs16", ("glu", ct)], writes=[("glu", ct)])
                        cx.op("act", lambda e: e.activation(out=csq_t[:, 512:528], in_=cs16[:], func=AF.Square), reads=["cs16", "csq_t"], writes=["csq_t"])
                        if ct == 0:
                            cx.op("dve", lambda e: e.tensor_copy(out=csum[:, 512:528], in_=cs16[:]), reads=["cs16", "csum"], writes=["csum"])
                            cx.op("dve", lambda e: e.tensor_copy(out=csq[:, 512:528], in_=csq_t[:, 512:528]), reads=["csq_t", "csq"], writes=["csq"])
                        else:
                            cx.op("dve", lambda e: e.tensor_tensor(out=csum[:, 512:528], in0=csum[:, 512:528], in1=cs16[:], op=ALU.add), reads=["cs16", "csum"], writes=["csum"])
                            cx.op("dve", lambda e: e.tensor_tensor(out=csq[:, 512:528], in0=csq[:, 512:528], in1=csq_t[:, 512:528], op=ALU.add), reads=["csq_t", "csq"], writes=["csq"])

                def conv_pump():
                    while pend and len(built) < 2:
                        ct = pend.pop(0)
                        emit_build(ct)
                        built.append(ct)

                def conv_tick(force=False):
                    cst8["tick"] += 1
                    if built and (force or cst8["tick"] % 2 == 0):
                        emit_conv(built.pop(0))
                        conv_pump()

                def conv_step(n):
                    return

                def wstat_chunk(wv, wkey, rhs_buf, rhs_keys, tiles, evac, conv_n=0, roff=0):
                    for m in range(4):
                        for (off, n) in tiles:
                            pi = nextps()
                            cx.mm([lambda e, k=k, pi=pi, off=off, n=n, m=m: e.matmul(psb[pi][:, 0:n], lhsT=wv[:, k, m * 128:(m + 1) * 128], rhs=rhs_buf[:, k, roff + off:roff + off + n], start=(k == 0), stop=(k == KT - 1))
                                   for k in range(KT)],
                                  reads=wkey + rhs_keys, writes=[("ps", pi)], cost=KT * (max(n, 64) / 2400.0 + 0.005))
                            evac(m, off, n, pi)
                        conv_tick()

                for c in range(4):
                    wv, wkey = use_chunk(sched[("gate", g, c)])
                    def ev_gate(m, off, n, pi):
                        cx.op("act", lambda e: e.activation(out=sigbuf[:, m, off:off + n], in_=psb[pi][:, 0:n], func=AF.Sigmoid),
                              reads=[("ps", pi)], writes=[("sig", m)])
                    wstat_chunk(wv, wkey, xn, xnk, mt_vg, ev_gate, conv_n=(2 if c > 0 else 0)); release(sched[("gate", g, c)])
                    wv, wkey = use_chunk(sched[("val", g, c)])
                    def ev_val(m, off, n, pi, c=c):
                        ft = c * 4 + m
                        if off == 0:
                            cx.op("dve", lambda e: e.tensor_tensor(out=glu[:, ft, 32:544], in0=psb[pi][:, 0:512], in1=sigbuf[:, m, 0:512], op=ALU.mult),
                                  reads=[("ps", pi), ("sig", m)], writes=[("glu", ft)])
                            if g == 0:
                                cx.op("act", lambda e: e.activation(out=carry[:, ft, :], in_=glu[:, ft, 512:544], func=AF.Copy), reads=[("glu", ft)], writes=[("carry", ft)])
                            else:
                                cx.op("dve", lambda e: e.tensor_tensor(out=glu_last[:, ft, :], in0=psb[pi][:, 480:512], in1=sigbuf[:, m, 480:512], op=ALU.mult),
                                      reads=[("ps", pi), ("sig", m)], writes=[("glu_last", ft)])
                        else:
                            cx.op("dve", lambda e: e.tensor_tensor(out=glu_s[:, ft, :], in0=psb[pi][:, 0:16], in1=sigbuf[:, m, 512:528], op=ALU.mult),
                                  reads=[("ps", pi), ("sig", m)], writes=[("glu_s", ft)])
                            cx.op("dve", lambda e: e.tensor_tensor(out=glu[:, ft, 0:32], in0=psb[pi][:, 16:48], in1=sigbuf[:, m, 528:560], op=ALU.mult),
                                  reads=[("ps", pi), ("sig", m)], writes=[("glu", ft)])
                    wstat_chunk(wv, wkey, xn, xnk, mt_vg, ev_val, conv_n=(2 if c > 0 else 0)); release(sched[("val", g, c)])
                    pend.extend(range(c * 4, c * 4 + 4)); conv_pump()
                if DEBUG and g == 0:
                    cx.dma("sp", lambda e: e.dma_start(out=dbg["d_glu"][:, :], in_=glu[:].rearrange("p k n -> p (k n)")), reads=[("glu", ft) for ft in range(KT)])

                for c in range(4):
                    wv, wkey = use_chunk(sched[("v", g, c)])
                    for t in range(ntile):
                        off, M = tile_cols(t)
                        pi = nextps()
                        cx.mm([lambda e, k=k, pi=pi, off=off, M=M: e.matmul(psb[pi][0:M, :], lhsT=xn[:, k, off:off + M], rhs=wv[:, k, :], start=(k == 0), stop=(k == KT - 1))
                               for k in range(KT)], reads=wkey + xnk, writes=[("ps", pi)])
                        if t < 4:
                            gv = tt[(c * 5 + t) % 3]
                            gk = ("tt", (c * 5 + t) % 3)
                            cx.op("act", lambda e, gv=gv, pi=pi: e.activation(out=gv[:, 0:512], in_=psb[pi][:, :], func=AF.Gelu), reads=[("ps", pi)], writes=[gk])
                            cx.op("dve", lambda e, gv=gv, t=t, c=c: e.bn_stats(out=vst[:, t, c, :], in_=gv[:, 0:512]), reads=[gk], writes=[("vst", t, c)])
                            cx.op("dve", lambda e, gv=gv, t=t, c=c: e.tensor_copy(out=A_v[:, t, c * 512:(c + 1) * 512], in_=gv[:, 0:512]), reads=[gk], writes=[("vf", t)])
                        else:
                            cx.op("act", lambda e, pi=pi, c=c: e.activation(out=vs_f[:, c * 512:(c + 1) * 512], in_=psb[pi][0:NSC, :], func=AF.Gelu), reads=[("ps", pi)], writes=["hTb"])
                            cx.op("dve", lambda e, t=t, c=c: e.bn_stats(out=vst[0:NSC, t, c, :], in_=vs_f[:, c * 512:(c + 1) * 512]), reads=["hTb"], writes=[("vst", t, c)])
                        conv_tick()
                    release(sched[("v", g, c)])
                for t in range(ntile):
                    M = 128 if t < 4 else NSC
                    cx.op("dve", lambda e, t=t, M=M: e.bn_aggr(out=mv[0:M, t, :], in_=vst[0:M, t, :, :].rearrange("p c s -> p (c s)")),
                          reads=[("vst", t, c) for c in range(4)], writes=[("mv", t)])
                    cx.op("act", lambda e, t=t, M=M: e.activation(out=vrs[0:M, t:t + 1], in_=mv[0:M, t, 1:2], func=AF.Sqrt, bias=eps_c[0:M, :], scale=1.0),
                          reads=[("mv", t), "eps"], writes=[("vrs", t)])
                    cx.op("dve", lambda e, t=t, M=M: e.reciprocal(out=vrs[0:M, t:t + 1], in_=vrs[0:M, t:t + 1]), reads=[("vrs", t)], writes=[("vrs", t)])
                    if t < 4:
                        cx.op("dve", lambda e, t=t: e.tensor_scalar(out=A_v[:, t, :], in0=A_v[:, t, :], scalar1=mv[:, t, 0:1], scalar2=vrs[:, t:t + 1], op0=ALU.subtract, op1=ALU.mult),
                              reads=[("vf", t), ("mv", t), ("vrs", t)], writes=[("vf", t)])
                    else:
                        cx.op("dve", lambda e, t=t: e.tensor_scalar(out=vs_f[:, :], in0=vs_f[:, :], scalar1=mv[0:NSC, t, 0:1], scalar2=vrs[0:NSC, t:t + 1], op0=ALU.subtract, op1=ALU.mult),
                              reads=["hTb", ("mv", t), ("vrs", t)], writes=["hTb"])
                    conv_fill()
                if has_s:
                    pi = nextps()
                    cx.mm([lambda e, ft=ft, pi=pi: e.transpose(out=psb[pi][:, ft * 16:(ft + 1) * 16], in_=vs_f[:, ft * 128:(ft + 1) * 128], identity=ident_f[0:NSC, 0:NSC])
                           for ft in range(KT)], reads=["hTb", "cst"], writes=[("ps", pi)])
                    for ft in range(KT):
                        cx.op("dve", lambda e, ft=ft, pi=pi: e.tensor_scalar(out=vsT[:, ft, :], in0=psb[pi][:, ft * 16:(ft + 1) * 16], scalar1=col(LVG, ft), scalar2=col(LVB, ft), op0=ALU.mult, op1=ALU.add),
                              reads=[("ps", pi), "colv"], writes=[("vsT", ft)])
                        cx.op("dve", lambda e, ft=ft: e.tensor_scalar(out=ssT[:, ft, :], in0=vsT[:, ft, :], scalar1=col(WSD, ft), scalar2=col(BS0, ft), op0=ALU.mult, op1=ALU.add),
                              reads=[("vsT", ft), "colv"], writes=[("ssT", ft)])
                    pis = [nextps() for _ in range(4)]
                    cx.mm([lambda e, ft=ft: e.transpose(out=psb[pis[ft // 4]][0:NSC, (ft % 4) * 128:(ft % 4 + 1) * 128], in_=vsT[:, ft, :], identity=ident_f)
                           for ft in range(KT)], reads=[("vsT", ft) for ft in range(KT)] + ["cst"], writes=[("ps", p) for p in pis])
                    for q in range(4):
                        cx.op("act", lambda e, q=q: e.activation(out=osb[0:NSC, q * 512:(q + 1) * 512], in_=psb[pis[q]][0:NSC, :], func=AF.Copy),
                              reads=[("ps", pis[q])], writes=["hTb"])
                    cx.dma("sp", lambda e: e.dma_start(out=ncv[:, :], in_=osb[0:NSC, :]), reads=["hTb"])
                if DEBUG and g == 0:
                    cx.dma("sp", lambda e: e.dma_start(out=dbg["d_v"][:, :], in_=A_v[:].rearrange("p t n -> p (t n)")), reads=vkeys)

                while built or pend:
                    conv_tick(force=True)
                for (off, n) in mt:
                    p1 = nextps(); p2 = nextps()
                    cx.mm([lambda e, p1=p1, off=off, n=n: e.matmul(psb[p1][:, 0:n], lhsT=odiv[:], rhs=csum[:, off:off + n], start=True, stop=True)],
                          reads=["odiv", "csum"], writes=[("ps", p1)])
                    cx.mm([lambda e, p2=p2, off=off, n=n: e.matmul(psb[p2][:, 0:n], lhsT=odiv[:], rhs=csq[:, off:off + n], start=True, stop=True)],
                          reads=["odiv", "csq"], writes=[("ps", p2)])
                    cx.op("act", lambda e, p1=p1, off=off, n=n: e.activation(out=mean_bc[:, off:off + n], in_=psb[p1][:, 0:n], func=AF.Copy), reads=[("ps", p1)], writes=["mean_bc"])
                    cx.op("dve", lambda e, off=off, n=n: e.tensor_tensor(out=rstdc[:, off:off + n], in0=mean_bc[:, off:off + n], in1=mean_bc[:, off:off + n], op=ALU.mult),
                          reads=["mean_bc"], writes=["rstdc"])
                    cx.op("dve", lambda e, p2=p2, off=off, n=n: e.tensor_tensor(out=rstdc[:, off:off + n], in0=psb[p2][:, 0:n], in1=rstdc[:, off:off + n], op=ALU.subtract),
                          reads=[("ps", p2), "rstdc"], writes=["rstdc"])
                    cx.op("act", lambda e, off=off, n=n: e.activation(out=rstdc[:, off:off + n], in_=rstdc[:, off:off + n], func=AF.Sqrt, bias=eps_c[:], scale=1.0), reads=["rstdc", "eps"], writes=["rstdc"])
                    cx.op("dve", lambda e, off=off, n=n: e.reciprocal(out=rstdc[:, off:off + n], in_=rstdc[:, off:off + n]), reads=["rstdc"], writes=["rstdc"])
                for ct in range(KT):
                    tb = tt[1 + ct % 2]; tk = ("tt", 1 + ct % 2)
                    cx.op("dve", lambda e, ct=ct, tb=tb: e.tensor_tensor(out=tb[:, 0:NTg], in0=glu[:, ct, 32:32 + NTg], in1=mean_bc[:, 0:NTg], op=ALU.subtract),
                          reads=[("glu", ct), "mean_bc"], writes=[tk])
                    cx.op("dve", lambda e, tb=tb: e.tensor_tensor(out=tb[:, 0:NTg], in0=tb[:, 0:NTg], in1=rstdc[:, 0:NTg], op=ALU.mult), reads=[tk, "rstdc"], writes=[tk])
                    cx.op("act", lambda e, ct=ct, tb=tb: e.activation(out=glu[:, ct, 32:32 + NTg], in_=tb[:, 0:NTg], func=AF.Silu, bias=col(LCB, ct), scale=col(LCG, ct)),
                          reads=[tk, "colv"], writes=[("glu", ct)])
                cbk = [("glu", ct) for ct in range(KT)]
                if DEBUG and g == 0:
                    cx.dma("sp", lambda e: e.dma_start(out=dbg["d_cb"][:, :], in_=glu[:].rearrange("p k n -> p (k n)")), reads=cbk)

                for c in range(4):
                    wv, wkey = use_chunk(sched[("u", g, c)])
                    for m in range(4):
                        ft = c * 4 + m
                        pis_ = nextps()
                        cx.mm([lambda e, j=j, ft=ft, pis_=pis_: e.matmul(psb[pis_][:, j * 128:(j + 1) * 128], lhsT=A_v[:, j, ft * 128:(ft + 1) * 128], rhs=trilWT[:, ft // 2, :], start=True, stop=True)
                               for j in range(4)], reads=[("vf", j) for j in range(4)] + [("tril", ft // 2)], writes=[("ps", pis_)])
                        piu = nextps()
                        cx.mm([lambda e, k=k, piu=piu, m=m: e.matmul(psb[piu][:, :], lhsT=wv[:, k, m * 128:(m + 1) * 128], rhs=xn[:, k, 0:512], start=(k == 0), stop=(k == KT - 1))
                               for k in range(KT)], reads=wkey + xnk, writes=[("ps", piu)])
                        ug = tt[0]; t1 = tt[1]
                        cx.op("act", lambda e, piu=piu: e.activation(out=ug[:, 0:512], in_=psb[piu][:, :], func=AF.Gelu), reads=[("ps", piu)], writes=[("tt", 0)])
                        cx.op("dve", lambda e, pis_=pis_, ft=ft: e.scalar_tensor_tensor(
                            out=t1[:, 0:512].rearrange("p (j t) -> p j t", j=4), in0=psb[pis_][:, :].rearrange("p (j t) -> p j t", j=4), scalar=col(LVG, ft),
                            in1=Cm[:, ft:ft + 1, :].to_broadcast([128, 4, 128]), op0=ALU.mult, op1=ALU.add),
                            reads=[("ps", pis_), ("Cm", ft), "colv"], writes=[("tt", 1)])
                        cx.op("dve", lambda e, ft=ft: e.tensor_tensor(out=usT[:, ft, 0:512], in0=ug[:, 0:512], in1=t1[:, 0:512], op=ALU.mult),
                              reads=[("tt", 0), ("tt", 1)], writes=[("usT", ft)])
                        if has_s:
                            pq = nextps()
                            cx.mm([lambda e, k=k, pq=pq, m=m: e.matmul(psb[pq][:, 0:16], lhsT=wv[:, k, m * 128:(m + 1) * 128], rhs=xn[:, k, 512:528], start=(k == 0), stop=(k == KT - 1))
                                   for k in range(KT)], reads=wkey + xnk, writes=[("ps", pq)])
                            cx.op("act", lambda e, pq=pq: e.activation(out=tt[2][:, 0:16], in_=psb[pq][:, 0:16], func=AF.Gelu), reads=[("ps", pq)], writes=[("tt", 2)])
                            cx.op("dve", lambda e, ft=ft: e.tensor_tensor(out=usT[:, ft, 512:528], in0=tt[2][:, 0:16], in1=ssT[:, ft, :], op=ALU.mult),
                                  reads=[("tt", 2), ("ssT", ft), ("usT", ft)], writes=[("usT", ft)])
                        conv_fill()
                    release(sched[("u", g, c)])
                usk = [("usT", ft) for ft in range(KT)]
                if DEBUG and g == 0:
                    cx.dma("sp", lambda e: e.dma_start(out=dbg["d_us"][:, :], in_=usT[:].rearrange("p k n -> p (k n)")), reads=usk)
                makeys = [("ma", ft) for ft in range(KT)]
                cx.alias(makeys, vkeys)

                for c in range(4):
                    wv, wkey = use_chunk(sched[("ga", g, c)])
                    def ev_ga(m, off, n, pi):
                        cx.op("act", lambda e: e.activation(out=sigbuf[:, m, off:off + n], in_=psb[pi][:, 0:n], func=AF.Sigmoid), reads=[("ps", pi)], writes=[("sig", m)])
                    wstat_chunk(wv, wkey, xn, xnk, mt, ev_ga, conv_n=4); release(sched[("ga", g, c)])
                    wv, wkey = use_chunk(sched[("pa", g, c)])
                    def ev_pa(m, off, n, pi, c=c):
                        ft = c * 4 + m
                        cx.op("dve", lambda e: e.tensor_tensor(out=A_ma[:, ft, off:off + n], in0=psb[pi][:, 0:n], in1=sigbuf[:, m, off:off + n], op=ALU.mult),
                              reads=[("ps", pi), ("sig", m), ("ma", ft)], writes=[("ma", ft)])
                    wstat_chunk(wv, wkey, usT, usk, mt, ev_pa, conv_n=4); release(sched[("pa", g, c)])
                for c in range(4):
                    wv, wkey = use_chunk(sched[("gb", g, c)])
                    wstat_chunk(wv, wkey, xn, xnk, mt, ev_ga); release(sched[("gb", g, c)])
                    wv, wkey = use_chunk(sched[("pb", g, c)])
                    def ev_pb(m, off, n, pi, c=c):
                        ft = c * 4 + m
                        cx.op("dve", lambda e: e.tensor_tensor(out=tt[0][:, 0:n], in0=psb[pi][:, 0:n], in1=sigbuf[:, m, off:off + n], op=ALU.mult),
                              reads=[("ps", pi), ("sig", m)], writes=[("tt", 0)])
                        cx.op("dve", lambda e: e.tensor_tensor(out=usT[:, ft, off:off + n], in0=tt[0][:, 0:n], in1=A_ma[:, ft, off:off + n], op=ALU.add),
                              reads=[("tt", 0), ("ma", ft), ("usT", ft)], writes=[("usT", ft)])
                    wstat_chunk(wv, wkey, glu, cbk, mt, ev_pb, roff=32); release(sched[("pb", g, c)])
                if DEBUG and g == 0:
                    cx.dma("sp", lambda e: e.dma_start(out=dbg["d_mg"][:, :], in_=usT[:].rearrange("p k n -> p (k n)")), reads=usk)

                if g == 1:
                    pis = [nextps() for _ in range(4)]
                    cx.mm([lambda e, ft=ft: e.transpose(out=psb[pis[ft // 4]][0:32, (ft % 4) * 128:(ft % 4 + 1) * 128], in_=glu_last[:, ft, :], identity=ident_f)
                           for ft in range(KT)], reads=[("glu_last", ft) for ft in range(KT)] + ["cst"], writes=[("ps", p) for p in pis])
                    for q in range(4):
                        cx.op("act", lambda e, q=q: e.activation(out=osb[0:32, q * 512:(q + 1) * 512], in_=psb[pis[q]][0:32, :], func=AF.Copy), reads=[("ps", pis[q])], writes=["hTb"])
                    cx.dma("sp", lambda e: e.dma_start(out=ncp[:, :], in_=osb[2:32, :]), reads=["hTb"])
                else:
                    pis = [nextps() for _ in range(4)]
                    cx.mm([lambda e, ft=ft: e.transpose(out=psb[pis[ft // 4]][0:NSC, (ft % 4) * 128:(ft % 4 + 1) * 128], in_=glu_s[:, ft, :], identity=ident_f)
                           for ft in range(KT)], reads=[("glu_s", ft) for ft in range(KT)] + ["cst"], writes=[("ps", p) for p in pis])
                    for q in range(4):
                        cx.op("act", lambda e, q=q: e.activation(out=osb[0:NSC, q * 512:(q + 1) * 512], in_=psb[pis[q]][0:NSC, :], func=AF.Copy), reads=[("ps", pis[q])], writes=["hTb"])
                    cx.dma("sp", lambda e: e.dma_start(out=ncs[:, 29 * D:30 * D], in_=osb[0:NSC, :]), reads=["hTb"])
                    cx.dma("sp", lambda e: e.dma_start(out=ncs[:, 0:29 * D], in_=st[:, D:30 * D]))

                cx.alias([("h", 0), ("h", 1)], makeys)
                wvs = [use_chunk(sched[("wo", g, c)]) for c in range(4)]
                for t in range(ntile):
                    off, M = tile_cols(t)
                    gt = (g * 4 + t) if t < 4 else 8
                    row0 = (c0 + off) if t < 4 else NPC
                    hb = A_h[:, t % 2, :]
                    hk = ("h", t % 2)
                    for c in range(4):
                        wv, wkey = wvs[c]
                        pi = nextps()
                        cx.mm([lambda e, k=k, pi=pi, off=off, M=M, wv=wv: e.matmul(psb[pi][0:M, :], lhsT=usT[:, k, off:off + M], rhs=wv[:, k, :], start=(k == 0), stop=(k == KT - 1))
                               for k in range(KT)], reads=wkey + usk, writes=[("ps", pi)])
                        xc = xtc[c % 2]
                        cx.dma("sp", lambda e, xc=xc, row0=row0, M=M, c=c: e.dma_start(out=xc[0:M, :], in_=xtok[row0:row0 + M, c * 512:(c + 1) * 512]), writes=[("xtc", c % 2)])
                        cx.op("dve", lambda e, pi=pi, xc=xc, hb=hb, M=M, c=c: e.tensor_tensor(out=hb[0:M, c * 512:(c + 1) * 512], in0=psb[pi][0:M, :], in1=xc[0:M, :], op=ALU.add),
                              reads=[("ps", pi), ("xtc", c % 2), hk], writes=[hk])
                    cx.dma("sp", lambda e, hb=hb, gt=gt, M=M: e.dma_start(out=hscr[gt * 128:gt * 128 + M, :], in_=hb[0:M, :]), reads=[hk], writes=[("hscr", gt)])
                    hu = hub[0]; huk = ("hub", 0)
                    cx.op("act", lambda e, hb=hb, hu=hu, M=M, t=t: e.activation(out=hu[0:M, :], in_=hb[0:M, :], func=AF.Square, accum_out=ssq[0:M, t % 2:t % 2 + 1]),
                          reads=[hk], writes=[huk, ("ssq", t % 2)])
                    cx.op("act", lambda e, M=M, t=t, gt=gt: e.activation(out=rstd_h[0:M, gt:gt + 1], in_=ssq[0:M, t % 2:t % 2 + 1], func=AF.Sqrt, bias=eps_c[0:M, :], scale=1.0 / D),
                          reads=[("ssq", t % 2), "eps"], writes=[("rstd_h", gt)])
                    cx.op("dve", lambda e, M=M, gt=gt: e.reciprocal(out=rstd_h[0:M, gt:gt + 1], in_=rstd_h[0:M, gt:gt + 1]), reads=[("rstd_h", gt)], writes=[("rstd_h", gt)])
                    if M < 128:
                        cx.op("dve", lambda e, hu=hu: e.memset(hu[:, :], 0.0), reads=[huk], writes=[huk])
                    cx.op("act", lambda e, hb=hb, hu=hu, M=M, gt=gt: e.activation(out=hu[0:M, :], in_=hb[0:M, :], func=AF.Copy, scale=rstd_h[0:M, gt:gt + 1]),
                          reads=[hk, ("rstd_h", gt), huk], writes=[huk])
                    cx.dma("sp", lambda e, hu=hu, gt=gt: e.dma_start(out=hnscr[gt * 128:(gt + 1) * 128, :], in_=hu[:, :]), reads=[huk], writes=[("hnscr", gt)])
                    for q in range(4):
                        pi = nextps()
                        cx.mm([lambda e, j=j, pi=pi, hb=hb, M=M, q=q: e.transpose(out=psb[pi][:, j * 128:j * 128 + M], in_=hb[0:M, (q * 4 + j) * 128:(q * 4 + j + 1) * 128], identity=ident_f[0:M, 0:M])
                               for j in range(4)], reads=[hk, "cst"], writes=[("ps", pi)])
                        cx.op("act", lambda e, pi=pi, q=q: e.activation(out=hT[:, q * 4:q * 4 + 4, :].rearrange("p j t -> p (j t)"), in_=psb[pi][:, :], func=AF.Copy),
                              reads=[("ps", pi)], writes=["hTb"])
                    pi = nextps()
                    cx.mm([lambda e, k=k, pi=pi, M=M: e.matmul(psb[pi][0:M, 0:36], lhsT=hT[:, k, 0:M], rhs=wrg[:, k, :], start=(k == 0), stop=(k == KT - 1))
                           for k in range(KT)], reads=["hTb", "wrg"], writes=[("ps", pi)])
                    if M < 128:
                        cx.op("dve", lambda e, gt=gt: e.memset(lg[:, gt, :], 0.0), writes=[("lg", gt)])
                    cx.op("dve", lambda e, pi=pi, M=M, gt=gt: e.scalar_tensor_tensor(out=lg[0:M, gt, :], in0=psb[pi][0:M, 0:36], scalar=rstd_h[0:M, gt:gt + 1], in1=br_bc[0:M, :], op0=ALU.mult, op1=ALU.add),
                          reads=[("ps", pi), ("rstd_h", gt), "br_bc", ("lg", gt)], writes=[("lg", gt)])
                release(*[sched[("wo", g, c)] for c in range(4)])

        p2 = contextlib.ExitStack()
        with p2:
            names2 = []

            def sb2(name, shape, dt=F32):
                names2.append(name)
                return sb(name, shape, dt, p2)
            ring2 = sb2("ring2", [128, RING, CH], BF16)
            extra["ring2"] = ring2
            zero_b = sb2("zero_b", [128, D], BF16)
            rt = sb2("rt", [128, 9, 16])
            Eoh = sb2("Eoh", [128, 9, 2, 32], BF16)
            E1f = sb2("E1f", [128, 2, 32])
            rtmp = sb2("rtmp", [128, 64])
            posf = sb2("posf", [128, 9, 2])
            xb = [sb2("xb%d" % i, [128, D], BF16) for i in range(2)]
            xgT = [sb2("xgT%d" % i, [128, KT, 128], BF16) for i in range(2)]
            hmid = [sb2("hmid%d" % i, [128, DE], BF16) for i in range(2)]
            hmT = [sb2("hmT%d" % i, [128, 8, 128], BF16) for i in range(2)]
            sgt = [sb2("sgt%d" % i, [128, 512]) for i in range(2)]
            yb = [sb2("yb%d" % i, [128, D]) for i in range(2)]
            le = sb2("le", [128, 8]); t8 = sb2("t8", [128, 8]); eg4 = sb2("eg4", [128, 4]); oh = sb2("oh", [128, 2, 8])
            cx.global_barrier()
            cx.op("dve", lambda e: e.memset(zero_b[:], 0.0), writes=["zero_b"])

            for gt in range(9):
                L = lg[:, gt, :]
                R = rt[:, gt, :]
                k_ = ("rt", gt)
                def dv(fn, reads=(), writes=()):
                    cx.op("dve", fn, reads=list(reads), writes=list(writes))
                dv(lambda e, L=L, R=R: e.tensor_reduce(out=R[:, 0:1], in_=L[:, 0:4], axis=AX.X, op=ALU.max), [("lg", gt)], [k_])
                dv(lambda e, R=R: e.tensor_scalar(out=R[:, 1:2], in0=R[:, 0:1], scalar1=-1.0, scalar2=None, op0=ALU.mult), [k_], [k_])
                dv(lambda e, L=L, R=R: e.tensor_scalar(out=R[:, 4:8], in0=L[:, 0:4], scalar1=R[:, 0:1], scalar2=None, op0=ALU.is_equal), [k_, ("lg", gt)], [k_])
                cx.op("act", lambda e, L=L, R=R: e.activation(out=eg4[:], in_=L[:, 0:4], func=AF.Exp, bias=R[:, 1:2], scale=1.0, accum_out=R[:, 2:3]),
                      reads=[k_, ("lg", gt)], writes=["eg4", k_])
                dv(lambda e, R=R: e.reciprocal(out=R[:, 3:4], in_=R[:, 2:3]), [k_], [k_])
                dv(lambda e, L=L, R=R: e.tensor_scalar(out=le[:], in0=L[:, 4:12], scalar1=R[:, 4:5], scalar2=None, op0=ALU.mult), [k_, ("lg", gt)], ["le"])
                for gq in range(1, 4):
                    dv(lambda e, L=L, R=R, gq=gq: e.scalar_tensor_tensor(out=le[:], in0=L[:, 4 + 8 * gq:12 + 8 * gq], scalar=R[:, 4 + gq:5 + gq], in1=le[:], op0=ALU.mult, op1=ALU.add),
                       [k_, ("lg", gt), "le"], ["le"])
                dv(lambda e: e.max(out=t8[:], in_=le[:]), ["le"], ["t8"])
                dv(lambda e, R=R: e.tensor_copy(out=R[:, 8:10], in_=t8[:, 0:2]), ["t8", k_], [k_])
                dv(lambda e, R=R: e.tensor_scalar(out=R[:, 10:11], in0=R[:, 8:9], scalar1=-1.0, scalar2=None, op0=ALU.mult), [k_], [k_])
                cx.op("act", lambda e, R=R: e.activation(out=R[:, 11:12], in_=R[:, 9:10], func=AF.Exp, bias=R[:, 10:11], scale=1.0), reads=[k_], writes=[k_])
                dv(lambda e, R=R: e.tensor_scalar(out=R[:, 12:13], in0=R[:, 11:12], scalar1=1.0, scalar2=None, op0=ALU.add), [k_], [k_])
                dv(lambda e, R=R: e.reciprocal(out=R[:, 12:13], in_=R[:, 12:13]), [k_], [k_])
                dv(lambda e, R=R, gt=gt: e.tensor_tensor(out=eww[:, gt, 0:1], in0=R[:, 3:4], in1=R[:, 12:13], op=ALU.mult), [k_, "eww"], ["eww"])
                dv(lambda e, R=R, gt=gt: e.tensor_tensor(out=eww[:, gt, 1:2], in0=eww[:, gt, 0:1], in1=R[:, 11:12], op=ALU.mult), [k_, "eww"], ["eww"])
                dv(lambda e, R=R: e.tensor_scalar(out=oh[:, 0, :], in0=le[:], scalar1=R[:, 8:9], scalar2=None, op0=ALU.is_equal), [k_, "le", "oh"], ["oh"])
                dv(lambda e, R=R: e.tensor_scalar(out=oh[:, 1, :], in0=le[:], scalar1=R[:, 9:10], scalar2=None, op0=ALU.is_equal), [k_, "le", "oh"], ["oh"])
                for kk in range(2):
                    for gq in range(4):
                        dv(lambda e, R=R, kk=kk, gq=gq, gt=gt: e.tensor_scalar(out=E1f[:, kk, gq * 8:(gq + 1) * 8], in0=oh[:, kk, :], scalar1=R[:, 4 + gq:5 + gq], scalar2=tokvalid[:, gt:gt + 1], op0=ALU.mult, op1=ALU.mult),
                           [k_, "oh", "E1f", "cst"], ["E1f"])
                dv(lambda e, gt=gt: e.tensor_copy(out=Eoh[:, gt, :, :], in_=E1f[:]), ["E1f", ("Eoh", gt)], [("Eoh", gt)])
            for gt in range(9):
                pi = nextps()
                fns = []
                for j in range(gt):
                    for kk in range(2):
                        fns.append(lambda e, j=j, kk=kk, pi=pi: e.matmul(psb[pi][:, 0:32], lhsT=ones_b[:], rhs=Eoh[:, j, kk, :], start=(j == 0 and kk == 0), stop=False))
                for kk in range(2):
                    fns.append(lambda e, kk=kk, pi=pi, gt=gt: e.matmul(psb[pi][:, 0:32], lhsT=triU_b[:], rhs=Eoh[:, gt, kk, :], start=(gt == 0 and kk == 0), stop=(kk == 1)))
                cx.mm(fns, reads=[("Eoh", j) for j in range(gt + 1)] + ["ones_b", "triU_b"], writes=[("ps", pi)])
                for kk in range(2):
                    cx.op("dve", lambda e, pi=pi, gt=gt, kk=kk: e.tensor_tensor(out=rtmp[:, 0:32], in0=psb[pi][:, 0:32], in1=Eoh[:, gt, kk, :], op=ALU.mult),
                          reads=[("ps", pi), ("Eoh", gt), "rtmp"], writes=["rtmp"])
                    cx.op("dve", lambda e, gt=gt, kk=kk: e.tensor_reduce(out=rt[:, gt, 13 + kk:14 + kk], in_=rtmp[:, 0:32], axis=AX.X, op=ALU.add), reads=["rtmp", ("rt", gt)], writes=[("rt", gt)])
                    cx.op("dve", lambda e, gt=gt, kk=kk: e.tensor_tensor(out=rtmp[:, 32:64], in0=Eoh[:, gt, kk, :], in1=base_e, op=ALU.mult), reads=[("Eoh", gt), "cst", "rtmp"], writes=["rtmp"])
                    cx.op("dve", lambda e, gt=gt, kk=kk: e.tensor_reduce(out=posf[:, gt, kk:kk + 1], in_=rtmp[:, 32:64], axis=AX.X, op=ALU.add), reads=["rtmp", "posf"], writes=["posf"])
                    cx.op("dve", lambda e, gt=gt, kk=kk: e.tensor_tensor(out=posf[:, gt, kk:kk + 1], in0=posf[:, gt, kk:kk + 1], in1=rt[:, gt, 13 + kk:14 + kk], op=ALU.add), reads=["posf", ("rt", gt)], writes=["posf"])
                    cx.op("dve", lambda e, gt=gt, kk=kk: e.tensor_scalar(out=rtmp[:, 0:1], in0=rt[:, gt, 13 + kk:14 + kk], scalar1=float(CAP) - 0.5, scalar2=tokvalid[:, gt:gt + 1], op0=ALU.is_lt, op1=ALU.mult),
                          reads=[("rt", gt), "rtmp", "cst"], writes=["rtmp"])
                    cx.op("dve", lambda e, gt=gt, kk=kk: e.tensor_tensor(out=eww[:, gt, kk:kk + 1], in0=eww[:, gt, kk:kk + 1], in1=rtmp[:, 0:1], op=ALU.mult), reads=["rtmp", "eww"], writes=["eww"])
                    cx.op("dve", lambda e, gt=gt, kk=kk: e.tensor_tensor(out=posf[:, gt, kk:kk + 1], in0=posf[:, gt, kk:kk + 1], in1=trash_c, op=ALU.subtract), reads=["posf", "cst"], writes=["posf"])
                    cx.op("dve", lambda e, gt=gt, kk=kk: e.scalar_tensor_tensor(out=posf[:, gt, kk:kk + 1], in0=posf[:, gt, kk:kk + 1], scalar=rtmp[:, 0:1], in1=trash_c, op0=ALU.mult, op1=ALU.add),
                          reads=["posf", "rtmp", "cst"], writes=["posf"])
            cx.op("dve", lambda e: e.tensor_copy(out=posu[:], in_=posf[:]), reads=["posf", "posu"], writes=["posu"])
            if DEBUG:
                cx.dma("sp", lambda e: e.dma_start(out=dbg["d_lg"][:, :], in_=lg[:].rearrange("p t n -> p (t n)")), reads=[("lg", gt) for gt in range(9)])
                cx.op("dve", lambda e: e.tensor_copy(out=rt[:, :, 0:2], in_=posf[:]), reads=["posf"] + [("rt", gt) for gt in range(9)], writes=[("rt", gt) for gt in range(9)])
                cx.op("dve", lambda e: e.tensor_copy(out=rt[:, :, 2:4], in_=eww[:]), reads=["eww"] + [("rt", gt) for gt in range(9)], writes=[("rt", gt) for gt in range(9)])
                cx.dma("sp", lambda e: e.dma_start(out=dbg["d_rt"][:, :].rearrange("p (t n) -> p t n", t=9), in_=rt[:, :, 0:8]), reads=[("rt", gt) for gt in range(9)])

            for gt in range(9):
                xbt = xb[gt % 2]
                cx.dma("sp", lambda e, xbt=xbt, gt=gt: e.dma_start(out=xbt[:, :], in_=hnscr[gt * 128:(gt + 1) * 128, :]), reads=[("hnscr", gt)], writes=[("xb", gt % 2)])
                for kk in range(2):
                    cx.dma("pool", lambda e, xbt=xbt, gt=gt, kk=kk: e.indirect_dma_start(
                        out=xg[:, :], out_offset=bass.IndirectOffsetOnAxis(ap=posu[:, gt, kk:kk + 1], axis=0), in_=xbt[:, :], in_offset=None, bounds_check=NSLOT + 127, oob_is_err=False),
                        reads=[("xb", gt % 2), "posu", "xg"], writes=[("xgs", gt, kk)])
            cx.alias(["xg"], [("xgs", gt, kk) for gt in range(9) for kk in range(2)])
            cx.dma("sp", lambda e: e.dma_start(out=yscr[NSLOT:NSLOT + 128, :], in_=zero_b[:].bitcast(F32).rearrange("p n -> p n")) if False else e.dma_start(out=yscr[NSLOT:NSLOT + 128, 0:D // 2], in_=zero_b[:].bitcast(F32)),
                   reads=["zero_b"], writes=[("yscr", NE)])
            cx.dma("sp", lambda e: e.dma_start(out=yscr[NSLOT:NSLOT + 128, D // 2:D], in_=zero_b[:].bitcast(F32)), reads=["zero_b"], writes=[("yscr", NE + 1)])

            def emit_ple_loads():
                for q in range(4):
                    pool_dma(lambda e, q=q: e.dma_start(out=wpg_t[:, :, q * 512:(q + 1) * 512], in_=wsrc(w_pg, q * 512, 512, KT)), writes=[("wpgq", q)])
                pool_dma(lambda e: e.dma_start(out=wpp_t[:], in_=w_pp.rearrange("(k p) n -> p k n", p=128)), writes=["wpp"])
                pool_dma(lambda e: e.dma_start(out=pTb[:], in_=pT.rearrange("(k p) n -> p k n", p=128)), writes=["pTb"])
                cx.dma("sp", lambda e: e.dma_start(out=gfin_bc[:], in_=gfin[0:1, :].partition_broadcast(128)), writes=["gfin_bc"])

            for ex in range(NE):
                b = ex % 2
                cx.dma("sp", lambda e, ex=ex, b=b: e.dma_start(out=xb[b][:, :], in_=xg[ex * 128:(ex + 1) * 128, :]), reads=["xg"], writes=[("xb", b)])
                for q in range(2):
                    pi = nextps()
                    pv = psb[pi][:, :].bitcast(BF16)
                    cx.mm([lambda e, j=j, pv=pv, q=q, b=b: e.transpose(out=pv[:, j * 128:(j + 1) * 128], in_=xb[b][:, (q * 8 + j) * 128:(q * 8 + j + 1) * 128], identity=ident_b[:])
                           for j in range(8)], reads=[("xb", b), "ident_b"], writes=[("ps", pi)])
                    cx.op("dve", lambda e, pv=pv, q=q, b=b: e.tensor_tensor(
                        out=xgT[b][:, q * 8:(q + 1) * 8, :], in0=pv[:, :].rearrange("p (j t) -> p j t", j=8),
                        in1=colv_t[:, GF * KT + q * 8:GF * KT + (q + 1) * 8].rearrange("p (j o) -> p j o", o=1).to_broadcast([128, 8, 128]), op=ALU.mult),
                        reads=[("ps", pi), "colv", ("xgT", b)], writes=[("xgT", b)], cost=1.2)
                for c in range(2):
                    wg, wgk = use_chunk(sched[("eg", ex, c)])
                    wu, wuk = use_chunk(sched[("eu", ex, c)])
                    if ex == NE - 1 and c == 1:
                        pass
                    pg_ = nextps(); pu_ = nextps()
                    cx.mm([lambda e, k=k, pg_=pg_, wg=wg, b=b: e.matmul(psb[pg_][:, :], lhsT=xgT[b][:, k, :], rhs=wg[:, k, :], start=(k == 0), stop=(k == KT - 1)) for k in range(KT)],
                          reads=wgk + [("xgT", b)], writes=[("ps", pg_)])
                    cx.mm([lambda e, k=k, pu_=pu_, wu=wu, b=b: e.matmul(psb[pu_][:, :], lhsT=xgT[b][:, k, :], rhs=wu[:, k, :], start=(k == 0), stop=(k == KT - 1)) for k in range(KT)],
                          reads=wuk + [("xgT", b)], writes=[("ps", pu_)])
                    cx.op("act", lambda e, pg_=pg_, c=c: e.activation(out=sgt[c][:, :], in_=psb[pg_][:, :], func=AF.Silu), reads=[("ps", pg_)], writes=[("sgt", c)])
                    cx.op("dve", lambda e, pu_=pu_, c=c, b=b: e.tensor_tensor(out=hmid[b][:, c * 512:(c + 1) * 512], in0=psb[pu_][:, :], in1=sgt[c][:, :], op=ALU.mult),
                          reads=[("ps", pu_), ("sgt", c), ("hmid", b)], writes=[("hmid", b)])
                    release(sched[("eg", ex, c)], sched[("eu", ex, c)])
                pi = nextps()
                pv = psb[pi][:, :].bitcast(BF16)
                cx.mm([lambda e, j=j, pv=pv, b=b: e.transpose(out=pv[:, j * 128:(j + 1) * 128], in_=hmid[b][:, j * 128:(j + 1) * 128], identity=ident_b[:]) for j in range(8)],
                      reads=[("hmid", b), "ident_b"], writes=[("ps", pi)])
                cx.op("dve", lambda e, pv=pv, b=b: e.tensor_copy(out=hmT[b][:].rearrange("p k t -> p (k t)"), in_=pv[:, :]), reads=[("ps", pi)], writes=[("hmT", b)])
                for c in range(2):
                    wd, wdk = use_chunk(sched[("ed", ex, c)])
                    for n in range(2):
                        pi = nextps()
                        cx.mm([lambda e, k=k, pi=pi, wd=wd, n=n, b=b: e.matmul(psb[pi][:, :], lhsT=hmT[b][:, k, :], rhs=wd[:, k, n * 512:(n + 1) * 512], start=(k == 0), stop=(k == 7)) for k in range(8)],
                              reads=wdk + [("hmT", b)], writes=[("ps", pi)])
                        cc = c * 2 + n
                        if cc % 2 == 0:
                            cx.op("act", lambda e, pi=pi, cc=cc, b=b: e.activation(out=yb[b][:, cc * 512:(cc + 1) * 512], in_=psb[pi][:, :], func=AF.Copy), reads=[("ps", pi), ("yb", b)], writes=[("yb", b)])
                        else:
                            cx.op("dve", lambda e, pi=pi, cc=cc, b=b: e.tensor_copy(out=yb[b][:, cc * 512:(cc + 1) * 512], in_=psb[pi][:, :]), reads=[("ps", pi), ("yb", b)], writes=[("yb", b)])
                cx.dma("sp", lambda e, ex=ex, b=b: e.dma_start(out=yscr[ex * 128:(ex + 1) * 128, :], in_=yb[b][:, :]), reads=[("yb", b)], writes=[("yscr", ex)])
                release(sched[("ed", ex, 0)], sched[("ed", ex, 1)])
            cx.alias(["yscr"], [("yscr", i) for i in range(NE + 2)])

        p3 = contextlib.ExitStack()
        with p3:
            def sb3(name, shape, dt=F32):
                return sb(name, shape, dt, p3)
            wpg_t = sb3("wpg_t", [128, KT, D], BF16)
            wpp_t = sb3("wpp_t", [128, 2, D], BF16)
            pTb = sb3("pTb", [128, 2, NTOK], BF16)
            gfin_bc = sb3("gfin_bc", [128, D])
            rf = [ring[:, s, :].bitcast(F32) for s in range(RING)]
            hcb = [sb3("hcb0", [128, D]), rf[0][:, 0:D]]
            y0 = [sb3("y00", [128, D]), rf[0][:, D:2 * D]]
            y1 = [sb3("y10", [128, D]), rf[1][:, 0:D]]
            yb = [sb3("yo0", [128, D]), rf[1][:, D:2 * D]]
            hu2 = sb3("hu2", [128, D], BF16)
            hT2 = sb3("hT2", [128, KT, 128], BF16)
            sq2 = sb3("sq2", [128, D], BF16)
            ss2a = sb3("ss2", [128, 2, 4])
            sgt = [sb3("sgtb%d" % i, [128, 512]) for i in range(2)]
            cx.global_barrier()
            emit_ple_loads()
            def ple_load(gt):
                b = gt % 2
                hc = hcb[b]
                cx.dma("sp", lambda e: e.dma_start(out=hc[:, :], in_=hscr[gt * 128:(gt + 1) * 128, :]), reads=[("hscr", gt)], writes=[("hcb", b)])
                cx.dma("pool", lambda e: e.indirect_dma_start(out=y0[b][:, :], out_offset=None, in_=yscr[:, :], in_offset=bass.IndirectOffsetOnAxis(ap=posu[:, gt, 0:1], axis=0), bounds_check=NSLOT + 127, oob_is_err=False),
                       reads=["yscr", "posu"], writes=[("y0", b)])
                cx.dma("pool", lambda e: e.indirect_dma_start(out=y1[b][:, :], out_offset=None, in_=yscr[:, :], in_offset=bass.IndirectOffsetOnAxis(ap=posu[:, gt, 1:2], axis=0), bounds_check=NSLOT + 127, oob_is_err=False),
                       reads=["yscr", "posu"], writes=[("y1", b)])
            hu2b = [hu2, ring[:, 2, 0:D]]
            hT2b = [hT2, ring[:, 2, D:2 * D].rearrange("p (k t) -> p k t", k=KT)]
            sq2b = [sq2, ring[:, 2, 2 * D:3 * D]]

            def ple_front(gt):
                b = gt % 2
                ss2 = ss2a[:, b, :]
                hc = hcb[b]
                cx.op("dve", lambda e: e.scalar_tensor_tensor(out=hc[:, :], in0=y0[b][:, :], scalar=eww[:, gt, 0:1], in1=hc[:, :], op0=ALU.mult, op1=ALU.add),
                      reads=[("y0", b), "eww", ("hcb", b)], writes=[("hcb", b)], cost=2.2)
                cx.op("dve", lambda e: e.scalar_tensor_tensor(out=hc[:, :], in0=y1[b][:, :], scalar=eww[:, gt, 1:2], in1=hc[:, :], op0=ALU.mult, op1=ALU.add),
                      reads=[("y1", b), "eww", ("hcb", b)], writes=[("hcb", b)], cost=2.2)
                cx.op("act", lambda e: e.activation(out=sq2b[b][:, :], in_=hc[:, :], func=AF.Square, accum_out=ss2[:, 0:1]), reads=[("hcb", b), ("sq2", b), ("ss2", b)], writes=[("sq2", b), ("ss2", b)])
                cx.op("act", lambda e: e.activation(out=ss2[:, 1:2], in_=ss2[:, 0:1], func=AF.Sqrt, bias=eps_c[:], scale=1.0 / D), reads=[("ss2", b), "eps"], writes=[("ss2", b)])
                cx.op("dve", lambda e: e.reciprocal(out=ss2[:, 1:2], in_=ss2[:, 1:2]), reads=[("ss2", b)], writes=[("ss2", b)])
                cx.op("act", lambda e: e.activation(out=hu2b[b][:, :], in_=hc[:, :], func=AF.Copy, scale=ss2[:, 1:2]), reads=[("hcb", b), ("ss2", b), ("hu2", b)], writes=[("hu2", b)])
                for q in range(2):
                    pi = nextps()
                    pv = psb[pi][:, :].bitcast(BF16)
                    cx.mm([lambda e, j=j, pv=pv, q=q: e.transpose(out=pv[:, j * 128:(j + 1) * 128], in_=hu2b[b][:, (q * 8 + j) * 128:(q * 8 + j + 1) * 128], identity=ident_b[:]) for j in range(8)],
                          reads=[("hu2", b), "ident_b"], writes=[("ps", pi)])
                    cx.op("dve", lambda e, pv=pv, q=q: e.tensor_tensor(
                        out=hT2b[b][:, q * 8:(q + 1) * 8, :], in0=pv[:, :].rearrange("p (j t) -> p j t", j=8),
                        in1=colv2_t[:, q * 8:(q + 1) * 8].rearrange("p (j o) -> p j o", o=1).to_broadcast([128, 8, 128]), op=ALU.mult),
                        reads=[("ps", pi), "colv2", ("hT2", b)], writes=[("hT2", b)], cost=1.2)

            def ple_mid_back(gt):
                b = gt % 2
                ss2 = ss2a[:, b, :]
                hc = hcb[b]
                M = 128 if gt < 8 else NSC
                row0 = gt * 128 if gt < 8 else NPC
                for n in range(4):
                    pg_ = nextps(); pp_ = nextps()
                    cx.mm([lambda e, k=k, pg_=pg_, n=n: e.matmul(psb[pg_][0:M, :], lhsT=hT2b[b][:, k, 0:M], rhs=wpg_t[:, k, n * 512:(n + 1) * 512], start=(k == 0), stop=(k == KT - 1)) for k in range(KT)],
                          reads=[("hT2", b), ("wpgq", n)], writes=[("ps", pg_)])
                    cx.mm([lambda e, k=k, pp_=pp_, n=n: e.matmul(psb[pp_][0:M, :], lhsT=pTb[:, k, row0:row0 + M], rhs=wpp_t[:, k, n * 512:(n + 1) * 512], start=(k == 0), stop=(k == 1)) for k in range(2)],
                          reads=["pTb", "wpp"], writes=[("ps", pp_)])
                    cx.op("act", lambda e, pg_=pg_: e.activation(out=sgt[0][0:M, :], in_=psb[pg_][0:M, :], func=AF.Sigmoid), reads=[("ps", pg_)], writes=[("sgt", 0)])
                    cx.op("dve", lambda e, pp_=pp_: e.tensor_tensor(out=sgt[1][0:M, :], in0=psb[pp_][0:M, :], in1=sgt[0][0:M, :], op=ALU.mult), reads=[("ps", pp_), ("sgt", 0)], writes=[("sgt", 1)])
                    cx.op("dve", lambda e, n=n: e.tensor_tensor(out=hc[0:M, n * 512:(n + 1) * 512], in0=hc[0:M, n * 512:(n + 1) * 512], in1=sgt[1][0:M, :], op=ALU.add),
                          reads=[("sgt", 1), ("hcb", b)], writes=[("hcb", b)])
                cx.op("act", lambda e: e.activation(out=sq2b[b][0:M, :], in_=hc[0:M, :], func=AF.Square, accum_out=ss2[0:M, 2:3]), reads=[("hcb", b), ("sq2", b), ("ss2", b)], writes=[("sq2", b), ("ss2", b)])
                cx.op("act", lambda e: e.activation(out=ss2[0:M, 3:4], in_=ss2[0:M, 2:3], func=AF.Sqrt, bias=eps_c[0:M, :], scale=1.0 / D), reads=[("ss2", b), "eps"], writes=[("ss2", b)])
                cx.op("dve", lambda e: e.reciprocal(out=ss2[0:M, 3:4], in_=ss2[0:M, 3:4]), reads=[("ss2", b)], writes=[("ss2", b)])
                yo = yb[b]
                cx.op("dve", lambda e: e.scalar_tensor_tensor(out=yo[0:M, :], in0=hc[0:M, :], scalar=ss2[0:M, 3:4], in1=gfin_bc[0:M, :], op0=ALU.mult, op1=ALU.mult),
                      reads=[("hcb", b), ("ss2", b), "gfin_bc", ("yb", b)], writes=[("yb", b)], cost=2.2)
                cx.dma("sp", lambda e: e.dma_start(out=y[row0:row0 + M, :], in_=yo[0:M, :]), reads=[("yb", b)], writes=[("yout", gt)])

            ple_load(0)
            ple_load(1)
            ple_front(0)
            for gt in range(9):
                if gt + 1 < 9:
                    ple_front(gt + 1)
                ple_mid_back(gt)
                if gt + 2 < 9:
                    ple_load(gt + 2)
            cx.final_wait()
    return nc


_CACHE = {}


def _cols(v):
    return np.ascontiguousarray(np.asarray(v, np.float32).reshape(KT, 128).T)


def kernel(x_prompt, x_sample, state_conv, p_prompt, p_sample, norm_mix, w_in, ln_v_g, ln_v_b,
           w_spatial, b_spatial, w_proj_a, conv_w, conv_b, ln_c_g, ln_c_b, w_proj_b, w_out,
           norm_ffn, w_router_group, b_router_group, w_router_expert, b_router_expert,
           w_exp_gate, w_exp_up, w_exp_down, norm_ple, w_ple_gate, w_ple_proj, final_norm):
    f = lambda a: np.ascontiguousarray(np.asarray(a, dtype=np.float32))
    x_prompt = f(x_prompt); x_sample = f(x_sample); state_conv = f(state_conv)
    p_prompt = f(p_prompt); p_sample = f(p_sample)
    if "nc" not in _CACHE:
        _CACHE["nc"] = build_program()
    nc = _CACHE["nc"]

    ws = f(w_spatial)[0]
    wsd = np.repeat(ws[:, 0, 0], 256)
    bs0 = np.repeat(f(b_spatial)[0][:, 0], 256)
    colv = np.concatenate([_cols(f(norm_mix)[0]), _cols(f(ln_v_g)[0]), _cols(f(ln_v_b)[0]), _cols(f(conv_b)[0]),
                           _cols(f(ln_c_g)[0]), _cols(f(ln_c_b)[0]), _cols(wsd), _cols(bs0), _cols(f(norm_ffn)[0])], axis=1)
    colv2 = _cols(f(norm_ple)[0])
    cwh = np.ascontiguousarray(f(conv_w)[0].T.reshape(KT, 128, 31).transpose(1, 0, 2).reshape(128, KT * 31))
    wsT = np.ascontiguousarray(ws.transpose(2, 0, 1).reshape(128, 8 * 128))
    bsr = f(b_spatial)[0].reshape(1, 8 * 128)
    wr = np.ascontiguousarray(np.concatenate([f(w_router_group)[0], f(w_router_expert)[0]], axis=1))
    brr = np.concatenate([f(b_router_group)[0], f(b_router_expert)[0]]).reshape(1, 36)
    ident = np.eye(128, dtype=np.float32)
    ii = np.arange(128)
    maskT = (ii[:, None] <= ii[None, :]).astype(np.float32)
    triU = (ii[:, None] < ii[None, :]).astype(np.float32)
    base_e = np.tile((np.arange(32, dtype=np.float32) * CAP)[None, :], (128, 1))
    trash = (NSLOT + ii).astype(np.float32)[:, None]
    tokvalid = np.ones((128, 9), np.float32)
    tokvalid[NSC:, 8] = 0.0
    cst = np.ascontiguousarray(np.concatenate([ident, maskT, triU, base_e, trash, tokvalid], axis=1))

    shared = dict(
        w_in=f(w_in)[0], w_pa=f(w_proj_a)[0], w_pb=f(w_proj_b)[0], w_out=f(w_out)[0], w_pg=f(w_ple_gate)[0],
        w_pp=f(w_ple_proj)[0], w_eg=f(w_exp_gate)[0], w_eu=f(w_exp_up)[0], w_ed=f(w_exp_down)[0],
        wr=wr, br=brr, colv=np.ascontiguousarray(colv), colv2=colv2, cw=cwh, wsT=wsT, bs=bsr,
        gfin=f(final_norm).reshape(1, D), cst=cst)
    in_maps = []
    for c in range(NCORES):
        b, half = c // 2, c % 2
        xp = x_prompt[b, half * NPC:(half + 1) * NPC]
        xs = x_sample[c * NSC:(c + 1) * NSC, 0]
        xt = np.concatenate([xp, xs], axis=0)
        if half == 1:
            xhal = x_prompt[b, NPC - 32:NPC]
        else:
            xhal = np.zeros((32, D), np.float32)
        pt = np.concatenate([p_prompt[0, b, half * NPC:(half + 1) * NPC], p_sample[0, c * NSC:(c + 1) * NSC, 0]], axis=0)
        stc = state_conv[0, c * NSC:(c + 1) * NSC]
        m = dict(shared)
        m.update(xT=np.ascontiguousarray(xt.T), xh=np.ascontiguousarray(xhal.T), xtok=np.ascontiguousarray(xt),
                 pT=np.ascontiguousarray(pt.T), stT=np.ascontiguousarray(stc.transpose(2, 0, 1).reshape(D, NSC * 30)),
                 st=np.ascontiguousarray(stc.reshape(NSC, 30 * D)))
        in_maps.append(m)
    res = run_bass_kernel_spmd(nc, in_maps, core_ids=list(range(NCORES)))
    rs = res.results
    _CACHE["last"] = rs
    y_prompt = np.zeros((4, 2048, D), np.float32)
    y_sample = np.zeros((128, 1, D), np.float32)
    ncp_o = np.zeros((1, 4, 30, D), np.float32)
    ncs_o = np.zeros((1, 128, 30, D), np.float32)
    ncv_o = np.zeros((1, 128, 1, D), np.float32)
    for c in range(NCORES):
        b, half = c // 2, c % 2
        yy = np.asarray(rs[c]["y"])
        y_prompt[b, half * NPC:(half + 1) * NPC] = yy[:NPC]
        y_sample[c * NSC:(c + 1) * NSC, 0] = yy[NPC:]
        if half == 1:
            ncp_o[0, b] = np.asarray(rs[c]["ncp"])
        ncs_o[0, c * NSC:(c + 1) * NSC] = np.asarray(rs[c]["ncs"]).reshape(NSC, 30, D)
        ncv_o[0, c * NSC:(c + 1) * NSC, 0] = np.asarray(rs[c]["ncv"])
    return (y_prompt, y_sample, ncp_o, ncs_o, ncv_o)
```

```python
import contextlib
import numpy as np
import concourse.bass as bass
import concourse.mybir as mybir
from concourse.bass_utils import run_bass_kernel_spmd

F32 = mybir.dt.float32
BF16 = mybir.dt.bfloat16
U32 = mybir.dt.uint32
AF = mybir.ActivationFunctionType
ALU = mybir.AluOpType
AX = mybir.AxisListType

NCORES = 8
D = 2048
KT = 16
NPC = 1024
NSC = 16
NTOK = NPC + NSC
NE = 32
DE = 1024
CAP = 128
NSLOT = NE * CAP
EPS = 1e-6
NL = 48
RING = 3
CH = 8192
DEBUG = False


def semkey(s):
    return getattr(s, "num", None) if getattr(s, "num", None) is not None else id(s)


class Ctx:
    def __init__(self, nc, es):
        self.nc = nc
        self.eng = dict(pe=nc.tensor, act=nc.scalar, dve=nc.vector, pool=nc.gpsimd, sp=nc.sync)
        self.csem = {e: es.enter_context(nc.semaphore("s_" + e)) for e in self.eng}
        self.ccnt = {e: 0 for e in self.eng}
        self.waited = {e: {} for e in self.eng}
        self.lanes = [es.enter_context(nc.semaphore("l%d" % i)) for i in range(NL)]
        self.lane_cnt = [0] * NL
        self.next_lane = 0
        self.lw = {}
        self.rd = {}
        self.t = {"pe": 0.0, "dve": 0.0}
        self.sems = {}
        for s in list(self.csem.values()) + self.lanes:
            self.sems[id(s)] = s

    def _wait(self, e, sid, val):
        if self.waited[e].get(sid, 0) >= val:
            return
        self.eng[e].wait_ge(self.sems[sid], val)
        self.waited[e][sid] = val

    def global_barrier(self):
        m = {}
        for en, s in self.csem.items():
            if self.ccnt[en]:
                m[id(s)] = self.ccnt[en]
        for i, s in enumerate(self.lanes):
            if self.lane_cnt[i]:
                m[id(s)] = self.lane_cnt[i]
        self.floor = {en: dict(m) for en in self.eng}

    def deps(self, e, reads, writes):
        fl = getattr(self, "floor", {}).get(e)
        if fl:
            for sid, v in fl.items():
                self._wait(e, sid, v)
            self.floor[e] = None
        need = {}
        for r in reads:
            for sid, v in self.lw.get(r, {}).items():
                need[sid] = max(need.get(sid, 0), v)
        for w in writes:
            for sid, v in self.lw.get(w, {}).items():
                need[sid] = max(need.get(sid, 0), v)
            for sid, v in self.rd.get(w, {}).items():
                need[sid] = max(need.get(sid, 0), v)
        for sid, v in need.items():
            self._wait(e, sid, v)

    def done(self, sid, val, reads, writes):
        for w in writes:
            self.lw[w] = {sid: val}
            self.rd[w] = {}
        for r in reads:
            if r in writes:
                continue
            d = self.rd.setdefault(r, {})
            d[sid] = max(d.get(sid, 0), val)

    def alias(self, new_keys, old_keys):
        m = {}
        for k in old_keys:
            for sid, v in self.lw.get(k, {}).items():
                m[sid] = max(m.get(sid, 0), v)
            for sid, v in self.rd.get(k, {}).items():
                m[sid] = max(m.get(sid, 0), v)
        for k in new_keys:
            cur = dict(self.lw.get(k, {}))
            for sid, v in m.items():
                cur[sid] = max(cur.get(sid, 0), v)
            self.lw[k] = cur

    def barrier_keys(self, new_keys):
        m = {}
        for e, s in self.csem.items():
            if self.ccnt[e]:
                m[id(s)] = self.ccnt[e]
        for i, s in enumerate(self.lanes):
            if self.lane_cnt[i]:
                m[id(s)] = self.lane_cnt[i]
        for k in new_keys:
            self.lw[k] = dict(m)
            self.rd[k] = {}

    def op(self, e, fn, reads=(), writes=(), cost=0.65):
        if e == "dve":
            self.t["dve"] += cost
        self.deps(e, reads, writes)
        inst = fn(self.eng[e])
        self.ccnt[e] += 1
        inst.then_inc(self.csem[e], 1)
        self.done(id(self.csem[e]), self.ccnt[e], reads, writes)

    def mm(self, fns, reads=(), writes=(), cost=None):
        self.t["pe"] += (0.215 * len(fns)) if cost is None else cost
        self.deps("pe", reads, writes)
        inst = None
        for fn in fns:
            inst = fn(self.eng["pe"])
        self.ccnt["pe"] += 1
        inst.then_inc(self.csem["pe"], 1)
        self.done(id(self.csem["pe"]), self.ccnt["pe"], reads, writes)

    def dma(self, e, fn, reads=(), writes=()):
        self.deps(e, reads, writes)
        L = self.next_lane
        self.next_lane = (L + 1) % NL
        if self.lane_cnt[L] > 0:
            self._wait(e, id(self.lanes[L]), self.lane_cnt[L])
        inst = fn(self.eng[e])
        self.lane_cnt[L] += 16
        inst.then_inc(self.lanes[L], 16)
        self.done(id(self.lanes[L]), self.lane_cnt[L], reads, writes)

    def final_wait(self):
        e = "sp"
        for i, s in enumerate(self.lanes):
            if self.lane_cnt[i]:
                self._wait(e, id(s), self.lane_cnt[i])
        for en, s in self.csem.items():
            if en != e and self.ccnt[en]:
                self._wait(e, id(s), self.ccnt[en])


def build_program():
    nc = bass.Bass("TRN2", target_bir_lowering=False)

    def din(name, shape, dt=F32):
        return nc.dram_tensor(name, list(shape), dt, kind="ExternalInput").ap()

    def dout(name, shape, dt=F32):
        return nc.dram_tensor(name, list(shape), dt, kind="ExternalOutput").ap()

    xT = din("xT", [D, NTOK])
    xh = din("xh", [D, 32])
    xtok = din("xtok", [NTOK, D])
    pT = din("pT", [256, NTOK])
    stT = din("stT", [D, NSC * 30])
    st = din("st", [NSC, 30 * D])
    w_in = din("w_in", [D, 6 * D])
    w_pa = din("w_pa", [D, D])
    w_pb = din("w_pb", [D, D])
    w_out = din("w_out", [D, D])
    w_pg = din("w_pg", [D, D])
    w_pp = din("w_pp", [256, D])
    w_eg = din("w_eg", [NE, D, DE])
    w_eu = din("w_eu", [NE, D, DE])
    w_ed = din("w_ed", [NE, DE, D])
    wr = din("wr", [D, 36])
    br = din("br", [1, 36])
    colv = din("colv", [128, 9 * KT])
    colv2 = din("colv2", [128, KT])
    cw = din("cw", [128, KT * 31])
    wsT = din("wsT", [128, 8 * 128])
    bs = din("bs", [1, 8 * 128])
    gfin = din("gfin", [1, D])
    cst = din("cst", [128, 128 * 3 + 32 + 1 + 9])

    y = dout("y", [NTOK, D])
    ncp = dout("ncp", [30, D])
    ncs = dout("ncs", [NSC, 30 * D])
    ncv = dout("ncv", [NSC, D])
    dbg = {}
    if DEBUG:
        dbg["d_xn"] = dout("d_xn", [128, KT * 560], BF16)
        dbg["d_glu"] = dout("d_glu", [128, KT * 560], BF16)
        dbg["d_v"] = dout("d_v", [128, 4 * D], BF16)
        dbg["d_us"] = dout("d_us", [128, KT * 528], BF16)
        dbg["d_cb"] = dout("d_cb", [128, KT * 560], BF16)
        dbg["d_mg"] = dout("d_mg", [128, KT * 528], BF16)
        dbg["d_lg"] = dout("d_lg", [128, 9 * 36])
        dbg["d_rt"] = dout("d_rt", [128, 9 * 8])

    hscr = nc.dram_tensor("hscr", [9 * 128, D], F32).ap()
    hnscr = nc.dram_tensor("hnscr", [9 * 128, D], BF16).ap()
    xg = nc.dram_tensor("xg", [NSLOT + 128, D], BF16).ap()
    yscr = nc.dram_tensor("yscr", [NSLOT + 128, D], F32).ap()

    es = contextlib.ExitStack()
    with es:
        cx = Ctx(nc, es)

        def sb(name, shape, dt=F32, stack=es):
            return stack.enter_context(nc.sbuf_tensor(name, list(shape), dt))

        psb = [es.enter_context(nc.psum_tensor("ps%d" % i, [128, 512], F32)) for i in range(8)]
        pstate = {"i": 0}

        def nextps():
            i = pstate["i"]
            pstate["i"] = (i + 1) % 8
            return i

        cst_t = sb("cst_t", [128, 128 * 3 + 32 + 1 + 9])
        colv_t = sb("colv_t", [128, 9 * KT])
        colv2_t = sb("colv2_t", [128, KT])
        cw_t = sb("cw_t", [128, KT, 31])
        eps_c = sb("eps_c", [128, 1])
        ident_b = sb("ident_b", [128, 128], BF16)
        ones_b = sb("ones_b", [128, 128], BF16)
        triU_b = sb("triU_b", [128, 128], BF16)
        odiv = sb("odiv", [128, 128])
        ring = sb("ring", [128, RING, CH], BF16)
        posu = sb("posu", [128, 9, 2], U32)
        eww = sb("eww", [128, 9, 2])
        lg = sb("lg", [128, 9, 36])
        rstd_h = sb("rstd_h", [128, 9])

        ident_f = cst_t[:, 0:128]
        maskT = cst_t[:, 128:256]
        triU_f = cst_t[:, 256:384]
        base_e = cst_t[:, 384:416]
        trash_c = cst_t[:, 416:417]
        tokvalid = cst_t[:, 417:426]

        def col(i, k, M=128):
            return colv_t[0:M, i * KT + k:i * KT + k + 1]
        GM, LVG, LVB, CVB, LCG, LCB, WSD, BS0, GF = range(9)

        cx.dma("sp", lambda e: e.dma_start(out=cst_t[:], in_=cst[:, :]), writes=["cst"])
        cx.dma("sp", lambda e: e.dma_start(out=colv_t[:], in_=colv[:, :]), writes=["colv"])
        cx.dma("sp", lambda e: e.dma_start(out=colv2_t[:], in_=colv2[:, :]), writes=["colv2"])
        cx.dma("sp", lambda e: e.dma_start(out=cw_t[:].rearrange("p k j -> p (k j)"), in_=cw[:, :]), writes=["cw"])
        cx.op("dve", lambda e: e.memset(eps_c[:], EPS), writes=["eps"])
        cx.op("dve", lambda e: e.memset(ones_b[:], 1.0), writes=["ones_b"])
        cx.op("dve", lambda e: e.memset(odiv[:], 1.0 / D), writes=["odiv"])
        cx.op("dve", lambda e: e.tensor_copy(out=ident_b[:], in_=ident_f), reads=["cst"], writes=["ident_b"])
        cx.op("dve", lambda e: e.tensor_copy(out=triU_b[:], in_=triU_f), reads=["cst"], writes=["triU_b"])


        chunks = []

        def wsrc(w2d, c0, ncols, kt):
            return w2d.rearrange("(k p) n -> p k n", p=128)[:, :, c0:c0 + ncols]

        wstate = {"next": 0}

        extra = {}
        slots = []

        def chunk_view(j):
            srcap, a, b = chunks[j]
            kind, slot = slots[j]
            if kind == "xn":
                v = extra["xn"][:].rearrange("p k n -> p (k n)")[:, 0:a * b].rearrange("p (k n) -> p k n", k=a)
                return v, [("xn", k) for k in range(KT)]
            if slot < RING:
                base = ring[:, slot, 0:a * b]
            else:
                base = extra["ring2"][:, slot - RING, 0:a * b]
            return base.rearrange("p (k n) -> p k n", k=a), [("ring", slot)]

        pool_tok = []
        MAXFLY = 3

        def pool_dma(fn, reads=(), writes=()):
            if len(pool_tok) >= MAXFLY:
                sid, val = pool_tok[-MAXFLY]
                cx._wait("pool", sid, val)
            cx.dma("pool", fn, reads=list(reads), writes=list(writes))
            L = (cx.next_lane - 1) % NL
            pool_tok.append((id(cx.lanes[L]), cx.lane_cnt[L]))

        def emit_wdma(j):
            srcap = chunks[j][0]
            dst, keys = chunk_view(j)
            pool_dma(lambda e: e.dma_start(out=dst, in_=srcap), writes=keys)

        released = set()
        preds = {}

        def allowed(n):
            if n >= phase2_start[0] and "ring2" not in extra and slots[n][1] >= RING:
                return False
            p = preds.get(n)
            return p is None or p in released

        def prefetch():
            while wstate["next"] < len(chunks) and allowed(wstate["next"]):
                emit_wdma(wstate["next"])
                wstate["next"] += 1

        def use_chunk(j):
            prefetch()
            assert j < wstate["next"], ("chunk not loadable", j, wstate["next"])
            v, keys = chunk_view(j)
            return v, keys

        def release(*js):
            for j in js:
                released.add(j)
            prefetch()

        sched = {}
        for g in range(2):
            for c in range(4):
                sched[("gate", g, c)] = len(chunks); chunks.append((wsrc(w_in, 3 * D + c * 512, 512, KT), KT, 512))
                sched[("val", g, c)] = len(chunks); chunks.append((wsrc(w_in, 2 * D + c * 512, 512, KT), KT, 512))
            for c in range(4):
                sched[("v", g, c)] = len(chunks); chunks.append((wsrc(w_in, D + c * 512, 512, KT), KT, 512))
            for c in range(4):
                sched[("u", g, c)] = len(chunks); chunks.append((wsrc(w_in, c * 512, 512, KT), KT, 512))
            for c in range(4):
                sched[("ga", g, c)] = len(chunks); chunks.append((wsrc(w_in, 4 * D + c * 512, 512, KT), KT, 512))
                sched[("pa", g, c)] = len(chunks); chunks.append((wsrc(w_pa, c * 512, 512, KT), KT, 512))
            for c in range(4):
                sched[("gb", g, c)] = len(chunks); chunks.append((wsrc(w_in, 5 * D + c * 512, 512, KT), KT, 512))
                sched[("pb", g, c)] = len(chunks); chunks.append((wsrc(w_pb, c * 512, 512, KT), KT, 512))
            for c in range(4):
                sched[("wo", g, c)] = len(chunks); chunks.append((wsrc(w_out, c * 512, 512, KT), KT, 512))
        for ex in range(NE):
            for c in range(2):
                sched[("eg", ex, c)] = len(chunks); chunks.append((wsrc(w_eg[ex], c * 512, 512, KT), KT, 512))
                sched[("eu", ex, c)] = len(chunks); chunks.append((wsrc(w_eu[ex], c * 512, 512, KT), KT, 512))
            for c in range(2):
                sched[("ed", ex, c)] = len(chunks); chunks.append((wsrc(w_ed[ex], c * 1024, 1024, 8), 8, 1024))

        phase2_start = [sched[("eg", 0, 0)]]
        ctr = 0
        for j in range(len(chunks)):
            if j < phase2_start[0]:
                isxn = any(j == sched[("wo", g, 3)] for g in range(2))
                if isxn:
                    slots.append(("xn", -1))
                else:
                    slots.append(("ring", ctr % RING)); ctr += 1
            else:
                slots.append(("ring", (j - phase2_start[0]) % (2 * RING)))
        lastown = {}
        for j in range(len(chunks)):
            if slots[j] in lastown:
                preds[j] = lastown[slots[j]]
            lastown[slots[j]] = j

        ms = contextlib.ExitStack()
        with ms:
            A = sb("A", [128, KT * 528], BF16, ms)
            xn = sb("xn", [128, KT, 560], BF16, ms)
            glu = sb("glu", [128, KT, 560], BF16, ms)
            xs = [sb("xs%d" % i, [128, 560], F32, ms) for i in range(2)]
            usT = sb("usT", [128, KT, 528], BF16, ms)
            sigbuf = sb("sigbuf", [128, 4, 560], F32, ms)
            carry = sb("carry", [128, KT, 32], BF16, ms)
            glu_sl = sb("glu_sl", [128, KT, 32], F32, ms)
            glu_s = glu_sl[:, :, 0:16]
            glu_last = glu_sl
            rstd_bc = sb("rstd_bc", [128, 560], F32, ms)
            sqt = [sb("sqt%d" % i, [128, 560], BF16, ms) for i in range(2)]
            cs16 = sb("cs16", [128, 16], F32, ms)
            dg = [sb("dg%d" % i, [128, 31, 128], BF16, ms) for i in range(2)]
            csq_t = sb("csq_t", [128, 528], F32, ms)
            csum = sb("csum", [128, 528], F32, ms)
            csq = sb("csq", [128, 528], F32, ms)
            mean_bc = sb("mean_bc", [128, 528], F32, ms)
            rstdc = sb("rstdc", [128, 528], F32, ms)
            tt = [sb("tt%d" % i, [128, 528], F32, ms) for i in range(3)]
            trilWT = sb("trilWT", [128, 8, 128], BF16, ms)
            Cm = sb("Cm", [128, KT, 128], BF16, ms)
            vst = sb("vst", [128, 5, 4, 6], F32, ms)
            mv = sb("mv", [128, 5, 2], F32, ms)
            vrs = sb("vrs", [128, 5], F32, ms)
            vsT = sb("vsT", [128, KT, 16], F32, ms)
            ssT = sb("ssT", [128, KT, 16], F32, ms)
            stc = [sb("stc0", [128, 16, 30], F32, ms)] * 2
            stm = sb("stm", [128, 16, 30], F32, ms)
            accs = sb("accs", [128, 16], F32, ms)
            xtc = [sb("xtc%d" % i, [128, 512], F32, ms) for i in range(2)]
            hub = [sb("hub0", [128, D], BF16, ms)] * 2
            hT = sb("hT", [128, KT, 128], F32, ms)
            hTf = hT[:].rearrange("p k t -> p (k t)")
            vs_f = hTf[0:NSC, :]
            osb = hTf[0:32, :]
            wsT_t = hT[:, 0:8, :]
            bs_bc = hT[:, 8:16, :]
            wrg = sb("wrg", [128, KT, 36], F32, ms)
            br_bc = sb("br_bc", [128, 36], F32, ms)
            ssq = sb("ssq", [128, 2], F32, ms)

            extra["xn"] = xn
            A_ma = A[:].rearrange("p (k n) -> p k n", k=KT)
            A_v = A[:, 0:4 * D].rearrange("p (t n) -> p t n", t=4)
            A_h = A[:, 0:4 * D].bitcast(F32).rearrange("p (t n) -> p t n", t=2)

            cx.dma("sp", lambda e: e.dma_start(out=hTf[:, 0:1024], in_=wsT[:, :]), writes=["hTb"])
            cx.dma("sp", lambda e: e.dma_start(out=hTf[:, 1024:2048], in_=bs[0:1, :].partition_broadcast(128)), reads=["hTb"], writes=["hTb"])
            cx.dma("sp", lambda e: e.dma_start(out=br_bc[:], in_=br[0:1, :].partition_broadcast(128)), writes=["br_bc"])
            cx.dma("sp", lambda e: e.dma_start(out=wrg[:], in_=wr.rearrange("(k p) n -> p k n", p=128)), writes=["wrg"])
            for gi in range(8):
                cx.op("dve", lambda e, gi=gi: e.tensor_tensor(out=trilWT[:, gi, :], in0=wsT_t[:, gi, :], in1=maskT, op=ALU.mult),
                      reads=["hTb", "cst"], writes=[("tril", gi)])
            for k in range(KT):
                cx.op("dve", lambda e, k=k: e.tensor_scalar(out=wrg[:, k, :], in0=wrg[:, k, :], scalar1=col(GF, k), scalar2=None, op0=ALU.mult),
                      reads=["wrg", "colv"], writes=["wrg"])
            for half in range(2):
                pi = nextps()
                cx.mm([lambda e, gi=gi, pi=pi: e.matmul(psb[pi][:, (gi % 4) * 128:(gi % 4 + 1) * 128], lhsT=ones_b[:], rhs=trilWT[:, gi, :], start=True, stop=True)
                       for gi in range(half * 4, half * 4 + 4)],
                      reads=["ones_b"] + [("tril", gi) for gi in range(half * 4, half * 4 + 4)], writes=[("ps", pi)])
                for ft in range(half * 8, half * 8 + 8):
                    gq = (ft // 2) % 4
                    cx.op("dve", lambda e, ft=ft, gq=gq, pi=pi: e.scalar_tensor_tensor(out=Cm[:, ft, :], in0=psb[pi][:, gq * 128:(gq + 1) * 128], scalar=col(LVB, ft), in1=bs_bc[:, ft // 2, :], op0=ALU.mult, op1=ALU.add),
                          reads=[("ps", pi), "hTb", "colv"], writes=[("Cm", ft)])

            for g in range(2):
                has_s = (g == 0)
                c0 = g * 512
                NTg = 528 if has_s else 512
                NXg = 560 if has_s else 512
                mt = [(0, 512)] + ([(512, 16)] if has_s else [])
                mt_vg = [(0, 512)] + ([(512, 48)] if has_s else [])
                ntile = 5 if has_s else 4

                def tile_cols(t):
                    return (t * 128, 128) if t < 4 else (512, 16)

                xTv = xT.rearrange("(k p) n -> p k n", p=128)
                xhv = xh.rearrange("(k p) n -> p k n", p=128)

                def load_x(k):
                    xb_ = xs[k % 2]
                    kk = ("xs", k % 2)
                    cx.dma("sp", lambda e: e.dma_start(out=xb_[:, 0:512], in_=xTv[:, k, c0:c0 + 512]), writes=[kk])
                    if has_s:
                        cx.dma("sp", lambda e: e.dma_start(out=xb_[:, 512:528], in_=xTv[:, k, NPC:NPC + 16]), reads=[kk], writes=[kk])
                        cx.dma("sp", lambda e: e.dma_start(out=xb_[:, 528:560], in_=xhv[:, k, :]), reads=[kk], writes=[kk])
                    return xb_, kk
                pa_ = nextps()
                pb_ = nextps() if has_s else None
                for k in range(KT):
                    xb_, kk = load_x(k)
                    cx.op("act", lambda e, k=k, xb_=xb_: e.activation(out=sqt[k % 2][:, 0:NXg], in_=xb_[:, 0:NXg], func=AF.Square),
                          reads=[kk], writes=[("sqt", k % 2)])
                    fns = [lambda e, k=k: e.matmul(psb[pa_][:, :], lhsT=ones_b[:], rhs=sqt[k % 2][:, 0:512], start=(k == 0), stop=(k == KT - 1))]
                    wr_ = [("ps", pa_)]
                    if has_s:
                        fns.append(lambda e, k=k: e.matmul(psb[pb_][:, 0:48], lhsT=ones_b[:], rhs=sqt[k % 2][:, 512:560], start=(k == 0), stop=(k == KT - 1)))
                        wr_.append(("ps", pb_))
                    cx.mm(fns, reads=[("sqt", k % 2), "ones_b"], writes=wr_)
                cx.op("act", lambda e: e.activation(out=rstd_bc[:, 0:512], in_=psb[pa_][:, :], func=AF.Sqrt, bias=eps_c[:], scale=1.0 / D),
                      reads=[("ps", pa_), "eps"], writes=["rstd_bc"])
                if has_s:
                    cx.op("act", lambda e: e.activation(out=rstd_bc[:, 512:560], in_=psb[pb_][:, 0:48], func=AF.Sqrt, bias=eps_c[:], scale=1.0 / D),
                          reads=[("ps", pb_), "eps", "rstd_bc"], writes=["rstd_bc"])
                cx.op("dve", lambda e: e.reciprocal(out=rstd_bc[:, 0:NXg], in_=rstd_bc[:, 0:NXg]), reads=["rstd_bc"], writes=["rstd_bc"])
                for k in range(KT):
                    xb_, kk = load_x(k)
                    cx.op("dve", lambda e, k=k, xb_=xb_: e.scalar_tensor_tensor(out=xn[:, k, 0:NXg], in0=xb_[:, 0:NXg], scalar=col(GM, k), in1=rstd_bc[:, 0:NXg], op0=ALU.mult, op1=ALU.mult),
                          reads=[kk, "rstd_bc", "colv"], writes=[("xn", k)])
                xnk = [("xn", k) for k in range(KT)]
                if DEBUG and g == 0:
                    cx.dma("sp", lambda e: e.dma_start(out=dbg["d_xn"][:, :], in_=xn[:].rearrange("p k n -> p (k n)")), reads=xnk)
                vkeys = [("vf", t) for t in range(4)]
                cx.alias(vkeys, ["A_h", ("h", 0), ("h", 1)])
                if g == 1:
                    for ft in range(KT):
                        cx.op("act", lambda e, ft=ft: e.activation(out=glu[:, ft, 0:32], in_=carry[:, ft, :], func=AF.Copy),
                              reads=[("carry", ft)], writes=[("glu", ft)])

                pend = []
                built = []
                cst8 = {"tick": 0}
                cstate = {"done": True}

                def conv_fill(slack=0.0):
                    return

                def emit_build(ct):
                    d = dg[ct % 2]
                    cx.op("dve", lambda e: e.tensor_tensor(out=d[:, :, :],
                                                           in0=ident_b[:].rearrange("p (o n) -> p o n", o=1).to_broadcast([128, 31, 128]),
                                                           in1=cw_t[:, ct, :].rearrange("p (j o) -> p j o", o=1).to_broadcast([128, 31, 128]), op=ALU.mult),
                          reads=["ident_b", "cw"], writes=[("dg", ct % 2)], cost=4.2)

                def emit_conv(ct):
                    d = dg[ct % 2]
                    pi = nextps()
                    cx.mm([lambda e, j=j: e.matmul(psb[pi][:, :], lhsT=d[:, j, :], rhs=glu[:, ct, 2 + j:514 + j], start=(j == 0), stop=(j == 30)) for j in range(31)],
                          reads=[("dg", ct % 2), ("glu", ct)], writes=[("ps", pi)])
                    T1 = xs[ct % 2]
                    tk1 = ("xs", ct % 2)
                    cx.op("act", lambda e: e.activation(out=T1[:, 0:512], in_=psb[pi][:, :], func=AF.Identity, bias=col(CVB, ct), scale=1.0),
                          reads=[("ps", pi), "colv"], writes=[tk1])
                    cx.op("act", lambda e: e.activation(out=csq_t[:, 0:512], in_=psb[pi][:, :], func=AF.Square, bias=col(CVB, ct), scale=1.0),
                          reads=[("ps", pi), "colv", "csq_t"], writes=["csq_t"])
                    cx.op("dve", lambda e: e.tensor_copy(out=glu[:, ct, 32:544], in_=T1[:, 0:512]), reads=[tk1, ("glu", ct)], writes=[("glu", ct)])
                    if ct == 0:
                        cx.op("dve", lambda e: e.tensor_copy(out=csum[:, 0:512], in_=T1[:, 0:512]), reads=[tk1], writes=["csum"])
                        cx.op("dve", lambda e: e.tensor_copy(out=csq[:, 0:512], in_=csq_t[:, 0:512]), reads=["csq_t"], writes=["csq"])
                    else:
                        cx.op("dve", lambda e: e.tensor_tensor(out=csum[:, 0:512], in0=csum[:, 0:512], in1=T1[:, 0:512], op=ALU.add), reads=[tk1, "csum"], writes=["csum"])
                        cx.op("dve", lambda e: e.tensor_tensor(out=csq[:, 0:512], in0=csq[:, 0:512], in1=csq_t[:, 0:512], op=ALU.add), reads=["csq_t", "csq"], writes=["csq"])
                    if has_s:
                        sc = stc[0]
                        sk = ("stc", 0)
                        cx.dma("sp", lambda e: e.dma_start(out=sc[:].rearrange("p b j -> p (b j)"), in_=stT[ct * 128:(ct + 1) * 128, :]), writes=[sk])
                        cx.op("dve", lambda e: e.tensor_tensor(out=stm[:], in0=sc[:], in1=cw_t[:, ct:ct + 1, 0:30].to_broadcast([128, 16, 30]), op=ALU.mult),
                              reads=[sk, "cw"], writes=["stm"])
                        cx.op("dve", lambda e: e.tensor_reduce(out=accs[:], in_=stm[:], axis=AX.X, op=ALU.add), reads=["stm"], writes=["accs"])
                        cx.op("dve", lambda e: e.scalar_tensor_tensor(out=accs[:], in0=glu_s[:, ct, :], scalar=cw_t[:, ct, 30:31], in1=accs[:], op0=ALU.mult, op1=ALU.add),
                              reads=[("glu_s", ct), "accs", "cw"], writes=["accs"])
                        cx.op("dve", lambda e: e.tensor_scalar(out=cs16[:], in0=accs[:], scalar1=col(CVB, ct), scalar2=None, op0=ALU.add), reads=["accs", "colv", "cs16"], writes=["cs16"])
                        cx.op("act", lambda e: e.activation(out=glu[:, ct, 544:560], in_=cs16[:], func=AF.Copy), reads=["cs16", ("glu", ct)], writes=[("glu", ct)])
                        cx.op("act", lambda e: e.activation(out=csq_t[:, 512:528], in_=cs16[:], func=AF.Square), reads=["cs16", "csq_t"], writes=["csq_t"])
                        if ct == 0:
                            cx.op("dve", lambda e: e.tensor_copy(out=csum[:, 512:528], in_=cs16[:]), reads=["cs16", "csum"], writes=["csum"])
                            cx.op("dve", lambda e: e.tensor_copy(out=csq[:, 512:528], in_=csq_t[:, 512:528]), reads=["csq_t", "csq"], writes=["csq"])
                        else:
                            cx.op("dve", lambda e: e.tensor_tensor(out=csum[:, 512:528], in0=csum[:, 512:528], in1=cs16[:], op=ALU.add), reads=["cs16", "csum"], writes=["csum"])
                            cx.op("dve", lambda e: e.tensor_tensor(out=csq[:, 512:528], in0=csq[:, 512:528], in1=csq_t[:, 512:528], op=ALU.add), reads=["csq_t", "csq"], writes=["csq"])

                def conv_pump():
                    while pend and len(built) < 2:
                        ct = pend.pop(0)
                        emit_build(ct)
                        built.append(ct)

                def conv_tick(force=False):
                    cst8["tick"] += 1
                    if built and (force or cst8["tick"] % 2 == 0):
                        emit_conv(built.pop(0))
                        conv_pump()

                def conv_step(n):
                    return

                def wstat_chunk(wv, wkey, rhs_buf, rhs_keys, tiles, evac, conv_n=0, roff=0):
                    for m in range(4):
                        for (off, n) in tiles:
                            pi = nextps()
                            cx.mm([lambda e, k=k, pi=pi, off=off, n=n, m=m: e.matmul(psb[pi][:, 0:n], lhsT=wv[:, k, m * 128:(m + 1) * 128], rhs=rhs_buf[:, k, roff + off:roff + off + n], start=(k == 0), stop=(k == KT - 1))
                                   for k in range(KT)],
                                  reads=wkey + rhs_keys, writes=[("ps", pi)], cost=KT * (max(n, 64) / 2400.0 + 0.005))
                            evac(m, off, n, pi)
                        conv_tick()

                for c in range(4):
                    wv, wkey = use_chunk(sched[("gate", g, c)])
                    def ev_gate(m, off, n, pi):
                        cx.op("act", lambda e: e.activation(out=sigbuf[:, m, off:off + n], in_=psb[pi][:, 0:n], func=AF.Sigmoid),
                              reads=[("ps", pi)], writes=[("sig", m)])
                    wstat_chunk(wv, wkey, xn, xnk, mt_vg, ev_gate, conv_n=(2 if c > 0 else 0)); release(sched[("gate", g, c)])
                    wv, wkey = use_chunk(sched[("val", g, c)])
                    def ev_val(m, off, n, pi, c=c):
                        ft = c * 4 + m
                        if off == 0:
                            cx.op("dve", lambda e: e.tensor_tensor(out=glu[:, ft, 32:544], in0=psb[pi][:, 0:512], in1=sigbuf[:, m, 0:512], op=ALU.mult),
                                  reads=[("ps", pi), ("sig", m)], writes=[("glu", ft)])
                            if g == 0:
                                cx.op("act", lambda e: e.activation(out=carry[:, ft, :], in_=glu[:, ft, 512:544], func=AF.Copy), reads=[("glu", ft)], writes=[("carry", ft)])
                            else:
                                cx.op("dve", lambda e: e.tensor_tensor(out=glu_last[:, ft, :], in0=psb[pi][:, 480:512], in1=sigbuf[:, m, 480:512], op=ALU.mult),
                                      reads=[("ps", pi), ("sig", m)], writes=[("glu_last", ft)])
                        else:
                            cx.op("dve", lambda e: e.tensor_tensor(out=glu_s[:, ft, :], in0=psb[pi][:, 0:16], in1=sigbuf[:, m, 512:528], op=ALU.mult),
                                  reads=[("ps", pi), ("sig", m)], writes=[("glu_s", ft)])
                            cx.op("dve", lambda e: e.tensor_tensor(out=glu[:, ft, 0:32], in0=psb[pi][:, 16:48], in1=sigbuf[:, m, 528:560], op=ALU.mult),
                                  reads=[("ps", pi), ("sig", m)], writes=[("glu", ft)])
                    wstat_chunk(wv, wkey, xn, xnk, mt_vg, ev_val, conv_n=(2 if c > 0 else 0)); release(sched[("val", g, c)])
                    pend.extend(range(c * 4, c * 4 + 4)); conv_pump()
                if DEBUG and g == 0:
                    cx.dma("sp", lambda e: e.dma_start(out=dbg["d_glu"][:, :], in_=glu[:].rearrange("p k n -> p (k n)")), reads=[("glu", ft) for ft in range(KT)])

                for c in range(4):
                    wv, wkey = use_chunk(sched[("v", g, c)])
                    for t in range(ntile):
                        off, M = tile_cols(t)
                        pi = nextps()
                        cx.mm([lambda e, k=k, pi=pi, off=off, M=M: e.matmul(psb[pi][0:M, :], lhsT=xn[:, k, off:off + M], rhs=wv[:, k, :], start=(k == 0), stop=(k == KT - 1))
                               for k in range(KT)], reads=wkey + xnk, writes=[("ps", pi)])
                        if t < 4:
                            gv = tt[(c * 5 + t) % 3]
                            gk = ("tt", (c * 5 + t) % 3)
                            cx.op("act", lambda e, gv=gv, pi=pi: e.activation(out=gv[:, 0:512], in_=psb[pi][:, :], func=AF.Gelu), reads=[("ps", pi)], writes=[gk])
                            cx.op("dve", lambda e, gv=gv, t=t, c=c: e.bn_stats(out=vst[:, t, c, :], in_=gv[:, 0:512]), reads=[gk], writes=[("vst", t, c)])
                            cx.op("dve", lambda e, gv=gv, t=t, c=c: e.tensor_copy(out=A_v[:, t, c * 512:(c + 1) * 512], in_=gv[:, 0:512]), reads=[gk], writes=[("vf", t)])
                        else:
                            cx.op("act", lambda e, pi=pi, c=c: e.activation(out=vs_f[:, c * 512:(c + 1) * 512], in_=psb[pi][0:NSC, :], func=AF.Gelu), reads=[("ps", pi)], writes=["hTb"])
                            cx.op("dve", lambda e, t=t, c=c: e.bn_stats(out=vst[0:NSC, t, c, :], in_=vs_f[:, c * 512:(c + 1) * 512]), reads=["hTb"], writes=[("vst", t, c)])
                        conv_tick()
                    release(sched[("v", g, c)])
                for t in range(ntile):
                    M = 128 if t < 4 else NSC
                    cx.op("dve", lambda e, t=t, M=M: e.bn_aggr(out=mv[0:M, t, :], in_=vst[0:M, t, :, :].rearrange("p c s -> p (c s)")),
                          reads=[("vst", t, c) for c in range(4)], writes=[("mv", t)])
                    cx.op("act", lambda e, t=t, M=M: e.activation(out=vrs[0:M, t:t + 1], in_=mv[0:M, t, 1:2], func=AF.Sqrt, bias=eps_c[0:M, :], scale=1.0),
                          reads=[("mv", t), "eps"], writes=[("vrs", t)])
                    cx.op("dve", lambda e, t=t, M=M: e.reciprocal(out=vrs[0:M, t:t + 1], in_=vrs[0:M, t:t + 1]), reads=[("vrs", t)], writes=[("vrs", t)])
                    if t < 4:
                        cx.op("dve", lambda e, t=t: e.tensor_scalar(out=A_v[:, t, :], in0=A_v[:, t, :], scalar1=mv[:, t, 0:1], scalar2=vrs[:, t:t + 1], op0=ALU.subtract, op1=ALU.mult),
                              reads=[("vf", t), ("mv", t), ("vrs", t)], writes=[("vf", t)])
                    else:
                        cx.op("dve", lambda e, t=t: e.tensor_scalar(out=vs_f[:, :], in0=vs_f[:, :], scalar1=mv[0:NSC, t, 0:1], scalar2=vrs[0:NSC, t:t + 1], op0=ALU.subtract, op1=ALU.mult),
                              reads=["hTb", ("mv", t), ("vrs", t)], writes=["hTb"])
                    conv_fill()
                if has_s:
                    pi = nextps()
                    cx.mm([lambda e, ft=ft, pi=pi: e.transpose(out=psb[pi][:, ft * 16:(ft + 1) * 16], in_=vs_f[:, ft * 128:(ft + 1) * 128], identity=ident_f[0:NSC, 0:NSC])
                           for ft in range(KT)], reads=["hTb", "cst"], writes=[("ps", pi)])
                    for ft in range(KT):
                        cx.op("dve", lambda e, ft=ft, pi=pi: e.tensor_scalar(out=vsT[:, ft, :], in0=psb[pi][:, ft * 16:(ft + 1) * 16], scalar1=col(LVG, ft), scalar2=col(LVB, ft), op0=ALU.mult, op1=ALU.add),
                              reads=[("ps", pi), "colv"], writes=[("vsT", ft)])
                        cx.op("dve", lambda e, ft=ft: e.tensor_scalar(out=ssT[:, ft, :], in0=vsT[:, ft, :], scalar1=col(WSD, ft), scalar2=col(BS0, ft), op0=ALU.mult, op1=ALU.add),
                              reads=[("vsT", ft), "colv"], writes=[("ssT", ft)])
                    pis = [nextps() for _ in range(4)]
                    cx.mm([lambda e, ft=ft: e.transpose(out=psb[pis[ft // 4]][0:NSC, (ft % 4) * 128:(ft % 4 + 1) * 128], in_=vsT[:, ft, :], identity=ident_f)
                           for ft in range(KT)], reads=[("vsT", ft) for ft in range(KT)] + ["cst"], writes=[("ps", p) for p in pis])
                    for q in range(4):
                        cx.op("act", lambda e, q=q: e.activation(out=osb[0:NSC, q * 512:(q + 1) * 512], in_=psb[pis[q]][0:NSC, :], func=AF.Copy),
                              reads=[("ps", pis[q])], writes=["hTb"])
                    cx.dma("sp", lambda e: e.dma_start(out=ncv[:, :], in_=osb[0:NSC, :]), reads=["hTb"])
                if DEBUG and g == 0:
                    cx.dma("sp", lambda e: e.dma_start(out=dbg["d_v"][:, :], in_=A_v[:].rearrange("p t n -> p (t n)")), reads=vkeys)

                while built or pend:
                    conv_tick(force=True)
                for (off, n) in mt:
                    p1 = nextps(); p2 = nextps()
                    cx.mm([lambda e, p1=p1, off=off, n=n: e.matmul(psb[p1][:, 0:n], lhsT=odiv[:], rhs=csum[:, off:off + n], start=True, stop=True)],
                          reads=["odiv", "csum"], writes=[("ps", p1)])
                    cx.mm([lambda e, p2=p2, off=off, n=n: e.matmul(psb[p2][:, 0:n], lhsT=odiv[:], rhs=csq[:, off:off + n], start=True, stop=True)],
                          reads=["odiv", "csq"], writes=[("ps", p2)])
                    cx.op("act", lambda e, p1=p1, off=off, n=n: e.activation(out=mean_bc[:, off:off + n], in_=psb[p1][:, 0:n], func=AF.Copy), reads=[("ps", p1)], writes=["mean_bc"])
                    cx.op("dve", lambda e, off=off, n=n: e.tensor_tensor(out=rstdc[:, off:off + n], in0=mean_bc[:, off:off + n], in1=mean_bc[:, off:off + n], op=ALU.mult),
                          reads=["mean_bc"], writes=["rstdc"])
                    cx.op("dve", lambda e, p2=p2, off=off, n=n: e.tensor_tensor(out=rstdc[:, off:off + n], in0=psb[p2][:, 0:n], in1=rstdc[:, off:off + n], op=ALU.subtract),
                          reads=[("ps", p2), "rstdc"], writes=["rstdc"])
                    cx.op("act", lambda e, off=off, n=n: e.activation(out=rstdc[:, off:off + n], in_=rstdc[:, off:off + n], func=AF.Sqrt, bias=eps_c[:], scale=1.0), reads=["rstdc", "eps"], writes=["rstdc"])
                    cx.op("dve", lambda e, off=off, n=n: e.reciprocal(out=rstdc[:, off:off + n], in_=rstdc[:, off:off + n]), reads=["rstdc"], writes=["rstdc"])
                for ct in range(KT):
                    tb = tt[1 + ct % 2]; tk = ("tt", 1 + ct % 2)
                    cx.op("dve", lambda e, ct=ct, tb=tb: e.tensor_tensor(out=tb[:, 0:NTg], in0=glu[:, ct, 32:32 + NTg], in1=mean_bc[:, 0:NTg], op=ALU.subtract),
                          reads=[("glu", ct), "mean_bc"], writes=[tk])
                    cx.op("dve", lambda e, tb=tb: e.tensor_tensor(out=tb[:, 0:NTg], in0=tb[:, 0:NTg], in1=rstdc[:, 0:NTg], op=ALU.mult), reads=[tk, "rstdc"], writes=[tk])
                    cx.op("act", lambda e, ct=ct, tb=tb: e.activation(out=glu[:, ct, 32:32 + NTg], in_=tb[:, 0:NTg], func=AF.Silu, bias=col(LCB, ct), scale=col(LCG, ct)),
                          reads=[tk, "colv"], writes=[("glu", ct)])
                cbk = [("glu", ct) for ct in range(KT)]
                if DEBUG and g == 0:
                    cx.dma("sp", lambda e: e.dma_start(out=dbg["d_cb"][:, :], in_=glu[:].rearrange("p k n -> p (k n)")), reads=cbk)

                for c in range(4):
                    wv, wkey = use_chunk(sched[("u", g, c)])
                    for m in range(4):
                        ft = c * 4 + m
                        pis_ = nextps()
                        cx.mm([lambda e, j=j, ft=ft, pis_=pis_: e.matmul(psb[pis_][:, j * 128:(j + 1) * 128], lhsT=A_v[:, j, ft * 128:(ft + 1) * 128], rhs=trilWT[:, ft // 2, :], start=True, stop=True)
                               for j in range(4)], reads=[("vf", j) for j in range(4)] + [("tril", ft // 2)], writes=[("ps", pis_)])
                        piu = nextps()
                        cx.mm([lambda e, k=k, piu=piu, m=m: e.matmul(psb[piu][:, :], lhsT=wv[:, k, m * 128:(m + 1) * 128], rhs=xn[:, k, 0:512], start=(k == 0), stop=(k == KT - 1))
                               for k in range(KT)], reads=wkey + xnk, writes=[("ps", piu)])
                        ug = tt[0]; t1 = tt[1]
                        cx.op("act", lambda e, piu=piu: e.activation(out=ug[:, 0:512], in_=psb[piu][:, :], func=AF.Gelu), reads=[("ps", piu)], writes=[("tt", 0)])
                        cx.op("dve", lambda e, pis_=pis_, ft=ft: e.scalar_tensor_tensor(
                            out=t1[:, 0:512].rearrange("p (j t) -> p j t", j=4), in0=psb[pis_][:, :].rearrange("p (j t) -> p j t", j=4), scalar=col(LVG, ft),
                            in1=Cm[:, ft:ft + 1, :].to_broadcast([128, 4, 128]), op0=ALU.mult, op1=ALU.add),
                            reads=[("ps", pis_), ("Cm", ft), "colv"], writes=[("tt", 1)])
                        cx.op("dve", lambda e, ft=ft: e.tensor_tensor(out=usT[:, ft, 0:512], in0=ug[:, 0:512], in1=t1[:, 0:512], op=ALU.mult),
                              reads=[("tt", 0), ("tt", 1)], writes=[("usT", ft)])
                        if has_s:
                            pq = nextps()
                            cx.mm([lambda e, k=k, pq=pq, m=m: e.matmul(psb[pq][:, 0:16], lhsT=wv[:, k, m * 128:(m + 1) * 128], rhs=xn[:, k, 512:528], start=(k == 0), stop=(k == KT - 1))
                                   for k in range(KT)], reads=wkey + xnk, writes=[("ps", pq)])
                            cx.op("act", lambda e, pq=pq: e.activation(out=tt[2][:, 0:16], in_=psb[pq][:, 0:16], func=AF.Gelu), reads=[("ps", pq)], writes=[("tt", 2)])
                            cx.op("dve", lambda e, ft=ft: e.tensor_tensor(out=usT[:, ft, 512:528], in0=tt[2][:, 0:16], in1=ssT[:, ft, :], op=ALU.mult),
                                  reads=[("tt", 2), ("ssT", ft), ("usT", ft)], writes=[("usT", ft)])
                        conv_fill()
                    release(sched[("u", g, c)])
                usk = [("usT", ft) for ft in range(KT)]
                if DEBUG and g == 0:
                    cx.dma("sp", lambda e: e.dma_start(out=dbg["d_us"][:, :], in_=usT[:].rearrange("p k n -> p (k n)")), reads=usk)
                makeys = [("ma", ft) for ft in range(KT)]
                cx.alias(makeys, vkeys)

                for c in range(4):
                    wv, wkey = use_chunk(sched[("ga", g, c)])
                    def ev_ga(m, off, n, pi):
                        cx.op("act", lambda e: e.activation(out=sigbuf[:, m, off:off + n], in_=psb[pi][:, 0:n], func=AF.Sigmoid), reads=[("ps", pi)], writes=[("sig", m)])
                    wstat_chunk(wv, wkey, xn, xnk, mt, ev_ga, conv_n=4); release(sched[("ga", g, c)])
                    wv, wkey = use_chunk(sched[("pa", g, c)])
                    def ev_pa(m, off, n, pi, c=c):
                        ft = c * 4 + m
                        cx.op("dve", lambda e: e.tensor_tensor(out=A_ma[:, ft, off:off + n], in0=psb[pi][:, 0:n], in1=sigbuf[:, m, off:off + n], op=ALU.mult),
                              reads=[("ps", pi), ("sig", m), ("ma", ft)], writes=[("ma", ft)])
                    wstat_chunk(wv, wkey, usT, usk, mt, ev_pa, conv_n=4); release(sched[("pa", g, c)])
                for c in range(4):
                    wv, wkey = use_chunk(sched[("gb", g, c)])
                    wstat_chunk(wv, wkey, xn, xnk, mt, ev_ga); release(sched[("gb", g, c)])
                    wv, wkey = use_chunk(sched[("pb", g, c)])
                    def ev_pb(m, off, n, pi, c=c):
                        ft = c * 4 + m
                        cx.op("dve", lambda e: e.tensor_tensor(out=tt[0][:, 0:n], in0=psb[pi][:, 0:n], in1=sigbuf[:, m, off:off + n], op=ALU.mult),
                              reads=[("ps", pi), ("sig", m)], writes=[("tt", 0)])
                        cx.op("dve", lambda e: e.tensor_tensor(out=usT[:, ft, off:off + n], in0=tt[0][:, 0:n], in1=A_ma[:, ft, off:off + n], op=ALU.add),
                              reads=[("tt", 0), ("ma", ft), ("usT", ft)], writes=[("usT", ft)])
                    wstat_chunk(wv, wkey, glu, cbk, mt, ev_pb, roff=32); release(sched[("pb", g, c)])
                if DEBUG and g == 0:
                    cx.dma("sp", lambda e: e.dma_start(out=dbg["d_mg"][:, :], in_=usT[:].rearrange("p k n -> p (k n)")), reads=usk)

                if g == 1:
                    pis = [nextps() for _ in range(4)]
                    cx.mm([lambda e, ft=ft: e.transpose(out=psb[pis[ft // 4]][0:32, (ft % 4) * 128:(ft % 4 + 1) * 128], in_=glu_last[:, ft, :], identity=ident_f)
                           for ft in range(KT)], reads=[("glu_last", ft) for ft in range(KT)] + ["cst"], writes=[("ps", p) for p in pis])
                    for q in range(4):
                        cx.op("act", lambda e, q=q: e.activation(out=osb[0:32, q * 512:(q + 1) * 512], in_=psb[pis[q]][0:32, :], func=AF.Copy), reads=[("ps", pis[q])], writes=["hTb"])
                    cx.dma("sp", lambda e: e.dma_start(out=ncp[:, :], in_=osb[2:32, :]), reads=["hTb"])
                else:
                    pis = [nextps() for _ in range(4)]
                    cx.mm([lambda e, ft=ft: e.transpose(out=psb[pis[ft // 4]][0:NSC, (ft % 4) * 128:(ft % 4 + 1) * 128], in_=glu_s[:, ft, :], identity=ident_f)
                           for ft in range(KT)], reads=[("glu_s", ft) for ft in range(KT)] + ["cst"], writes=[("ps", p) for p in pis])
                    for q in range(4):
                        cx.op("act", lambda e, q=q: e.activation(out=osb[0:NSC, q * 512:(q + 1) * 512], in_=psb[pis[q]][0:NSC, :], func=AF.Copy), reads=[("ps", pis[q])], writes=["hTb"])
                    cx.dma("sp", lambda e: e.dma_start(out=ncs[:, 29 * D:30 * D], in_=osb[0:NSC, :]), reads=["hTb"])
                    cx.dma("sp", lambda e: e.dma_start(out=ncs[:, 0:29 * D], in_=st[:, D:30 * D]))

                cx.alias([("h", 0), ("h", 1)], makeys)
                wvs = [use_chunk(sched[("wo", g, c)]) for c in range(4)]
                for t in range(ntile):
                    off, M = tile_cols(t)
                    gt = (g * 4 + t) if t < 4 else 8
                    row0 = (c0 + off) if t < 4 else NPC
                    hb = A_h[:, t % 2, :]
                    hk = ("h", t % 2)
                    for c in range(4):
                        wv, wkey = wvs[c]
                        pi = nextps()
                        cx.mm([lambda e, k=k, pi=pi, off=off, M=M, wv=wv: e.matmul(psb[pi][0:M, :], lhsT=usT[:, k, off:off + M], rhs=wv[:, k, :], start=(k == 0), stop=(k == KT - 1))
                               for k in range(KT)], reads=wkey + usk, writes=[("ps", pi)])
                        xc = xtc[c % 2]
                        cx.dma("sp", lambda e, xc=xc, row0=row0, M=M, c=c: e.dma_start(out=xc[0:M, :], in_=xtok[row0:row0 + M, c * 512:(c + 1) * 512]), writes=[("xtc", c % 2)])
                        cx.op("dve", lambda e, pi=pi, xc=xc, hb=hb, M=M, c=c: e.tensor_tensor(out=hb[0:M, c * 512:(c + 1) * 512], in0=psb[pi][0:M, :], in1=xc[0:M, :], op=ALU.add),
                              reads=[("ps", pi), ("xtc", c % 2), hk], writes=[hk])
                    cx.dma("sp", lambda e, hb=hb, gt=gt, M=M: e.dma_start(out=hscr[gt * 128:gt * 128 + M, :], in_=hb[0:M, :]), reads=[hk], writes=[("hscr", gt)])
                    hu = hub[0]; huk = ("hub", 0)
                    cx.op("act", lambda e, hb=hb, hu=hu, M=M, t=t: e.activation(out=hu[0:M, :], in_=hb[0:M, :], func=AF.Square, accum_out=ssq[0:M, t % 2:t % 2 + 1]),
                          reads=[hk], writes=[huk, ("ssq", t % 2)])
                    cx.op("act", lambda e, M=M, t=t, gt=gt: e.activation(out=rstd_h[0:M, gt:gt + 1], in_=ssq[0:M, t % 2:t % 2 + 1], func=AF.Sqrt, bias=eps_c[0:M, :], scale=1.0 / D),
                          reads=[("ssq", t % 2), "eps"], writes=[("rstd_h", gt)])
                    cx.op("dve", lambda e, M=M, gt=gt: e.reciprocal(out=rstd_h[0:M, gt:gt + 1], in_=rstd_h[0:M, gt:gt + 1]), reads=[("rstd_h", gt)], writes=[("rstd_h", gt)])
                    if M < 128:
                        cx.op("dve", lambda e, hu=hu: e.memset(hu[:, :], 0.0), reads=[huk], writes=[huk])
                    cx.op("act", lambda e, hb=hb, hu=hu, M=M, gt=gt: e.activation(out=hu[0:M, :], in_=hb[0:M, :], func=AF.Copy, scale=rstd_h[0:M, gt:gt + 1]),
                          reads=[hk, ("rstd_h", gt), huk], writes=[huk])
                    cx.dma("sp", lambda e, hu=hu, gt=gt: e.dma_start(out=hnscr[gt * 128:(gt + 1) * 128, :], in_=hu[:, :]), reads=[huk], writes=[("hnscr", gt)])
                    for q in range(4):
                        pi = nextps()
                        cx.mm([lambda e, j=j, pi=pi, hb=hb, M=M, q=q: e.transpose(out=psb[pi][:, j * 128:j * 128 + M], in_=hb[0:M, (q * 4 + j) * 128:(q * 4 + j + 1) * 128], identity=ident_f[0:M, 0:M])
                               for j in range(4)], reads=[hk, "cst"], writes=[("ps", pi)])
                        cx.op("act", lambda e, pi=pi, q=q: e.activation(out=hT[:, q * 4:q * 4 + 4, :].rearrange("p j t -> p (j t)"), in_=psb[pi][:, :], func=AF.Copy),
                              reads=[("ps", pi)], writes=["hTb"])
                    pi = nextps()
                    cx.mm([lambda e, k=k, pi=pi, M=M: e.matmul(psb[pi][0:M, 0:36], lhsT=hT[:, k, 0:M], rhs=wrg[:, k, :], start=(k == 0), stop=(k == KT - 1))
                           for k in range(KT)], reads=["hTb", "wrg"], writes=[("ps", pi)])
                    if M < 128:
                        cx.op("dve", lambda e, gt=gt: e.memset(lg[:, gt, :], 0.0), writes=[("lg", gt)])
                    cx.op("dve", lambda e, pi=pi, M=M, gt=gt: e.scalar_tensor_tensor(out=lg[0:M, gt, :], in0=psb[pi][0:M, 0:36], scalar=rstd_h[0:M, gt:gt + 1], in1=br_bc[0:M, :], op0=ALU.mult, op1=ALU.add),
                          reads=[("ps", pi), ("rstd_h", gt), "br_bc", ("lg", gt)], writes=[("lg", gt)])
                release(*[sched[("wo", g, c)] for c in range(4)])

        p2 = contextlib.ExitStack()
        with p2:
            names2 = []

            def sb2(name, shape, dt=F32):
                names2.append(name)
                return sb(name, shape, dt, p2)
            ring2 = sb2("ring2", [128, RING, CH], BF16)
            extra["ring2"] = ring2
            zero_b = sb2("zero_b", [128, D], BF16)
            rt = sb2("rt", [128, 9, 16])
            Eoh = sb2("Eoh", [128, 9, 2, 32], BF16)
            E1f = sb2("E1f", [128, 2, 32])
            rtmp = sb2("rtmp", [128, 64])
            posf = sb2("posf", [128, 9, 2])
            xb = [sb2("xb%d" % i, [128, D], BF16) for i in range(2)]
            xgT = [sb2("xgT%d" % i, [128, KT, 128], BF16) for i in range(2)]
            hmid = [sb2("hmid%d" % i, [128, DE], BF16) for i in range(2)]
            hmT = [sb2("hmT%d" % i, [128, 8, 128], BF16) for i in range(2)]
            sgt = [sb2("sgt%d" % i, [128, 512]) for i in range(2)]
            yb = [sb2("yb%d" % i, [128, D]) for i in range(2)]
            le = sb2("le", [128, 8]); t8 = sb2("t8", [128, 8]); eg4 = sb2("eg4", [128, 4]); oh = sb2("oh", [128, 2, 8])
            cx.global_barrier()
            cx.op("dve", lambda e: e.memset(zero_b[:], 0.0), writes=["zero_b"])
            prefetch()

            for gt in range(9):
                L = lg[:, gt, :]
                R = rt[:, gt, :]
                k_ = ("rt", gt)
                def dv(fn, reads=(), writes=()):
                    cx.op("dve", fn, reads=list(reads), writes=list(writes))
                dv(lambda e, L=L, R=R: e.tensor_reduce(out=R[:, 0:1], in_=L[:, 0:4], axis=AX.X, op=ALU.max), [("lg", gt)], [k_])
                dv(lambda e, R=R: e.tensor_scalar(out=R[:, 1:2], in0=R[:, 0:1], scalar1=-1.0, scalar2=None, op0=ALU.mult), [k_], [k_])
                dv(lambda e, L=L, R=R: e.tensor_scalar(out=R[:, 4:8], in0=L[:, 0:4], scalar1=R[:, 0:1], scalar2=None, op0=ALU.is_equal), [k_, ("lg", gt)], [k_])
                cx.op("act", lambda e, L=L, R=R: e.activation(out=eg4[:], in_=L[:, 0:4], func=AF.Exp, bias=R[:, 1:2], scale=1.0, accum_out=R[:, 2:3]),
                      reads=[k_, ("lg", gt)], writes=["eg4", k_])
                dv(lambda e, R=R: e.reciprocal(out=R[:, 3:4], in_=R[:, 2:3]), [k_], [k_])
                dv(lambda e, L=L, R=R: e.tensor_scalar(out=le[:], in0=L[:, 4:12], scalar1=R[:, 4:5], scalar2=None, op0=ALU.mult), [k_, ("lg", gt)], ["le"])
                for gq in range(1, 4):
                    dv(lambda e, L=L, R=R, gq=gq: e.scalar_tensor_tensor(out=le[:], in0=L[:, 4 + 8 * gq:12 + 8 * gq], scalar=R[:, 4 + gq:5 + gq], in1=le[:], op0=ALU.mult, op1=ALU.add),
                       [k_, ("lg", gt), "le"], ["le"])
                dv(lambda e: e.max(out=t8[:], in_=le[:]), ["le"], ["t8"])
                dv(lambda e, R=R: e.tensor_copy(out=R[:, 8:10], in_=t8[:, 0:2]), ["t8", k_], [k_])
                dv(lambda e, R=R: e.tensor_scalar(out=R[:, 10:11], in0=R[:, 8:9], scalar1=-1.0, scalar2=None, op0=ALU.mult), [k_], [k_])
                cx.op("act", lambda e, R=R: e.activation(out=R[:, 11:12], in_=R[:, 9:10], func=AF.Exp, bias=R[:, 10:11], scale=1.0), reads=[k_], writes=[k_])
                dv(lambda e, R=R: e.tensor_scalar(out=R[:, 12:13], in0=R[:, 11:12], scalar1=1.0, scalar2=None, op0=ALU.add), [k_], [k_])
                dv(lambda e, R=R: e.reciprocal(out=R[:, 12:13], in_=R[:, 12:13]), [k_], [k_])
                dv(lambda e, R=R, gt=gt: e.tensor_tensor(out=eww[:, gt, 0:1], in0=R[:, 3:4], in1=R[:, 12:13], op=ALU.mult), [k_, "eww"], ["eww"])
                dv(lambda e, R=R, gt=gt: e.tensor_tensor(out=eww[:, gt, 1:2], in0=eww[:, gt, 0:1], in1=R[:, 11:12], op=ALU.mult), [k_, "eww"], ["eww"])
                dv(lambda e, R=R: e.tensor_scalar(out=oh[:, 0, :], in0=le[:], scalar1=R[:, 8:9], scalar2=None, op0=ALU.is_equal), [k_, "le", "oh"], ["oh"])
                dv(lambda e, R=R: e.tensor_scalar(out=oh[:, 1, :], in0=le[:], scalar1=R[:, 9:10], scalar2=None, op0=ALU.is_equal), [k_, "le", "oh"], ["oh"])
                for kk in range(2):
                    for gq in range(4):
                        dv(lambda e, R=R, kk=kk, gq=gq, gt=gt: e.tensor_scalar(out=E1f[:, kk, gq * 8:(gq + 1) * 8], in0=oh[:, kk, :], scalar1=R[:, 4 + gq:5 + gq], scalar2=tokvalid[:, gt:gt + 1], op0=ALU.mult, op1=ALU.mult),
                           [k_, "oh", "E1f", "cst"], ["E1f"])
                dv(lambda e, gt=gt: e.tensor_copy(out=Eoh[:, gt, :, :], in_=E1f[:]), ["E1f", ("Eoh", gt)], [("Eoh", gt)])
            for gt in range(9):
                pi = nextps()
                fns = []
                for j in range(gt):
                    for kk in range(2):
                        fns.append(lambda e, j=j, kk=kk, pi=pi: e.matmul(psb[pi][:, 0:32], lhsT=ones_b[:], rhs=Eoh[:, j, kk, :], start=(j == 0 and kk == 0), stop=False))
                for kk in range(2):
                    fns.append(lambda e, kk=kk, pi=pi, gt=gt: e.matmul(psb[pi][:, 0:32], lhsT=triU_b[:], rhs=Eoh[:, gt, kk, :], start=(gt == 0 and kk == 0), stop=(kk == 1)))
                cx.mm(fns, reads=[("Eoh", j) for j in range(gt + 1)] + ["ones_b", "triU_b"], writes=[("ps", pi)])
                for kk in range(2):
                    cx.op("dve", lambda e, pi=pi, gt=gt, kk=kk: e.tensor_tensor(out=rtmp[:, 0:32], in0=psb[pi][:, 0:32], in1=Eoh[:, gt, kk, :], op=ALU.mult),
                          reads=[("ps", pi), ("Eoh", gt), "rtmp"], writes=["rtmp"])
                    cx.op("dve", lambda e, gt=gt, kk=kk: e.tensor_reduce(out=rt[:, gt, 13 + kk:14 + kk], in_=rtmp[:, 0:32], axis=AX.X, op=ALU.add), reads=["rtmp", ("rt", gt)], writes=[("rt", gt)])
                    cx.op("dve", lambda e, gt=gt, kk=kk: e.tensor_tensor(out=rtmp[:, 32:64], in0=Eoh[:, gt, kk, :], in1=base_e, op=ALU.mult), reads=[("Eoh", gt), "cst", "rtmp"], writes=["rtmp"])
                    cx.op("dve", lambda e, gt=gt, kk=kk: e.tensor_reduce(out=posf[:, gt, kk:kk + 1], in_=rtmp[:, 32:64], axis=AX.X, op=ALU.add), reads=["rtmp", "posf"], writes=["posf"])
                    cx.op("dve", lambda e, gt=gt, kk=kk: e.tensor_tensor(out=posf[:, gt, kk:kk + 1], in0=posf[:, gt, kk:kk + 1], in1=rt[:, gt, 13 + kk:14 + kk], op=ALU.add), reads=["posf", ("rt", gt)], writes=["posf"])
                    cx.op("dve", lambda e, gt=gt, kk=kk: e.tensor_scalar(out=rtmp[:, 0:1], in0=rt[:, gt, 13 + kk:14 + kk], scalar1=float(CAP) - 0.5, scalar2=tokvalid[:, gt:gt + 1], op0=ALU.is_lt, op1=ALU.mult),
                          reads=[("rt", gt), "rtmp", "cst"], writes=["rtmp"])
                    cx.op("dve", lambda e, gt=gt, kk=kk: e.tensor_tensor(out=eww[:, gt, kk:kk + 1], in0=eww[:, gt, kk:kk + 1], in1=rtmp[:, 0:1], op=ALU.mult), reads=["rtmp", "eww"], writes=["eww"])
                    cx.op("dve", lambda e, gt=gt, kk=kk: e.tensor_tensor(out=posf[:, gt, kk:kk + 1], in0=posf[:, gt, kk:kk + 1], in1=trash_c, op=ALU.subtract), reads=["posf", "cst"], writes=["posf"])
                    cx.op("dve", lambda e, gt=gt, kk=kk: e.scalar_tensor_tensor(out=posf[:, gt, kk:kk + 1], in0=posf[:, gt, kk:kk + 1], scalar=rtmp[:, 0:1], in1=trash_c, op0=ALU.mult, op1=ALU.add),
                          reads=["posf", "rtmp", "cst"], writes=["posf"])
            cx.op("dve", lambda e: e.tensor_copy(out=posu[:], in_=posf[:]), reads=["posf", "posu"], writes=["posu"])
            if DEBUG:
                cx.dma("sp", lambda e: e.dma_start(out=dbg["d_lg"][:, :], in_=lg[:].rearrange("p t n -> p (t n)")), reads=[("lg", gt) for gt in range(9)])
                cx.op("dve", lambda e: e.tensor_copy(out=rt[:, :, 0:2], in_=posf[:]), reads=["posf"] + [("rt", gt) for gt in range(9)], writes=[("rt", gt) for gt in range(9)])
                cx.op("dve", lambda e: e.tensor_copy(out=rt[:, :, 2:4], in_=eww[:]), reads=["eww"] + [("rt", gt) for gt in range(9)], writes=[("rt", gt) for gt in range(9)])
                cx.dma("sp", lambda e: e.dma_start(out=dbg["d_rt"][:, :].rearrange("p (t n) -> p t n", t=9), in_=rt[:, :, 0:8]), reads=[("rt", gt) for gt in range(9)])

            for gt in range(9):
                xbt = xb[gt % 2]
                cx.dma("sp", lambda e, xbt=xbt, gt=gt: e.dma_start(out=xbt[:, :], in_=hnscr[gt * 128:(gt + 1) * 128, :]), reads=[("hnscr", gt)], writes=[("xb", gt % 2)])
                for kk in range(2):
                    cx.dma("pool", lambda e, xbt=xbt, gt=gt, kk=kk: e.indirect_dma_start(
                        out=xg[:, :], out_offset=bass.IndirectOffsetOnAxis(ap=posu[:, gt, kk:kk + 1], axis=0), in_=xbt[:, :], in_offset=None, bounds_check=NSLOT + 127, oob_is_err=False),
                        reads=[("xb", gt % 2), "posu", "xg"], writes=[("xgs", gt, kk)])
            cx.alias(["xg"], [("xgs", gt, kk) for gt in range(9) for kk in range(2)])
            cx.dma("sp", lambda e: e.dma_start(out=yscr[NSLOT:NSLOT + 128, :], in_=zero_b[:].bitcast(F32).rearrange("p n -> p n")) if False else e.dma_start(out=yscr[NSLOT:NSLOT + 128, 0:D // 2], in_=zero_b[:].bitcast(F32)),
                   reads=["zero_b"], writes=[("yscr", NE)])
            cx.dma("sp", lambda e: e.dma_start(out=yscr[NSLOT:NSLOT + 128, D // 2:D], in_=zero_b[:].bitcast(F32)), reads=["zero_b"], writes=[("yscr", NE + 1)])

            def emit_ple_loads():
                for q in range(4):
                    pool_dma(lambda e, q=q: e.dma_start(out=wpg_t[:, :, q * 512:(q + 1) * 512], in_=wsrc(w_pg, q * 512, 512, KT)), writes=[("wpgq", q)])
                pool_dma(lambda e: e.dma_start(out=wpp_t[:], in_=w_pp.rearrange("(k p) n -> p k n", p=128)), writes=["wpp"])
                pool_dma(lambda e: e.dma_start(out=pTb[:], in_=pT.rearrange("(k p) n -> p k n", p=128)), writes=["pTb"])
                cx.dma("sp", lambda e: e.dma_start(out=gfin_bc[:], in_=gfin[0:1, :].partition_broadcast(128)), writes=["gfin_bc"])

            for ex in range(NE):
                b = ex % 2
                cx.dma("sp", lambda e, ex=ex, b=b: e.dma_start(out=xb[b][:, :], in_=xg[ex * 128:(ex + 1) * 128, :]), reads=["xg"], writes=[("xb", b)])
                for q in range(2):
                    pi = nextps()
                    pv = psb[pi][:, :].bitcast(BF16)
                    cx.mm([lambda e, j=j, pv=pv, q=q, b=b: e.transpose(out=pv[:, j * 128:(j + 1) * 128], in_=xb[b][:, (q * 8 + j) * 128:(q * 8 + j + 1) * 128], identity=ident_b[:])
                           for j in range(8)], reads=[("xb", b), "ident_b"], writes=[("ps", pi)])
                    cx.op("dve", lambda e, pv=pv, q=q, b=b: e.tensor_tensor(
                        out=xgT[b][:, q * 8:(q + 1) * 8, :], in0=pv[:, :].rearrange("p (j t) -> p j t", j=8),
                        in1=colv_t[:, GF * KT + q * 8:GF * KT + (q + 1) * 8].rearrange("p (j o) -> p j o", o=1).to_broadcast([128, 8, 128]), op=ALU.mult),
                        reads=[("ps", pi), "colv", ("xgT", b)], writes=[("xgT", b)], cost=1.2)
                for c in range(2):
                    wg, wgk = use_chunk(sched[("eg", ex, c)])
                    wu, wuk = use_chunk(sched[("eu", ex, c)])
                    if ex == NE - 1 and c == 1:
                        pass
                    pg_ = nextps(); pu_ = nextps()
                    cx.mm([lambda e, k=k, pg_=pg_, wg=wg, b=b: e.matmul(psb[pg_][:, :], lhsT=xgT[b][:, k, :], rhs=wg[:, k, :], start=(k == 0), stop=(k == KT - 1)) for k in range(KT)],
                          reads=wgk + [("xgT", b)], writes=[("ps", pg_)])
                    cx.mm([lambda e, k=k, pu_=pu_, wu=wu, b=b: e.matmul(psb[pu_][:, :], lhsT=xgT[b][:, k, :], rhs=wu[:, k, :], start=(k == 0), stop=(k == KT - 1)) for k in range(KT)],
                          reads=wuk + [("xgT", b)], writes=[("ps", pu_)])
                    cx.op("act", lambda e, pg_=pg_, c=c: e.activation(out=sgt[c][:, :], in_=psb[pg_][:, :], func=AF.Silu), reads=[("ps", pg_)], writes=[("sgt", c)])
                    cx.op("dve", lambda e, pu_=pu_, c=c, b=b: e.tensor_tensor(out=hmid[b][:, c * 512:(c + 1) * 512], in0=psb[pu_][:, :], in1=sgt[c][:, :], op=ALU.mult),
                          reads=[("ps", pu_), ("sgt", c), ("hmid", b)], writes=[("hmid", b)])
                    release(sched[("eg", ex, c)], sched[("eu", ex, c)])
                pi = nextps()
                pv = psb[pi][:, :].bitcast(BF16)
                cx.mm([lambda e, j=j, pv=pv, b=b: e.transpose(out=pv[:, j * 128:(j + 1) * 128], in_=hmid[b][:, j * 128:(j + 1) * 128], identity=ident_b[:]) for j in range(8)],
                      reads=[("hmid", b), "ident_b"], writes=[("ps", pi)])
                cx.op("dve", lambda e, pv=pv, b=b: e.tensor_copy(out=hmT[b][:].rearrange("p k t -> p (k t)"), in_=pv[:, :]), reads=[("ps", pi)], writes=[("hmT", b)])
                for c in range(2):
                    wd, wdk = use_chunk(sched[("ed", ex, c)])
                    for n in range(2):
                        pi = nextps()
                        cx.mm([lambda e, k=k, pi=pi, wd=wd, n=n, b=b: e.matmul(psb[pi][:, :], lhsT=hmT[b][:, k, :], rhs=wd[:, k, n * 512:(n + 1) * 512], start=(k == 0), stop=(k == 7)) for k in range(8)],
                              reads=wdk + [("hmT", b)], writes=[("ps", pi)])
                        cc = c * 2 + n
                        if cc % 2 == 0:
                            cx.op("act", lambda e, pi=pi, cc=cc, b=b: e.activation(out=yb[b][:, cc * 512:(cc + 1) * 512], in_=psb[pi][:, :], func=AF.Copy), reads=[("ps", pi), ("yb", b)], writes=[("yb", b)])
                        else:
                            cx.op("dve", lambda e, pi=pi, cc=cc, b=b: e.tensor_copy(out=yb[b][:, cc * 512:(cc + 1) * 512], in_=psb[pi][:, :]), reads=[("ps", pi), ("yb", b)], writes=[("yb", b)])
                cx.dma("sp", lambda e, ex=ex, b=b: e.dma_start(out=yscr[ex * 128:(ex + 1) * 128, :], in_=yb[b][:, :]), reads=[("yb", b)], writes=[("yscr", ex)])
                release(sched[("ed", ex, 0)], sched[("ed", ex, 1)])
            cx.alias(["yscr"], [("yscr", i) for i in range(NE + 2)])

        p3 = contextlib.ExitStack()
        with p3:
            def sb3(name, shape, dt=F32):
                return sb(name, shape, dt, p3)
            wpg_t = sb3("wpg_t", [128, KT, D], BF16)
            wpp_t = sb3("wpp_t", [128, 2, D], BF16)
            pTb = sb3("pTb", [128, 2, NTOK], BF16)
            gfin_bc = sb3("gfin_bc", [128, D])
            rf = [ring[:, s, :].bitcast(F32) for s in range(RING)]
            hcb = [sb3("hcb0", [128, D]), rf[0][:, 0:D]]
            y0 = [sb3("y00", [128, D]), rf[0][:, D:2 * D]]
            y1 = [sb3("y10", [128, D]), rf[1][:, 0:D]]
            yb = [sb3("yo0", [128, D]), rf[1][:, D:2 * D]]
            hu2 = sb3("hu2", [128, D], BF16)
            hT2 = sb3("hT2", [128, KT, 128], BF16)
            sq2 = sb3("sq2", [128, D], BF16)
            ss2a = sb3("ss2", [128, 2, 4])
            sgt = [sb3("sgtb%d" % i, [128, 512]) for i in range(2)]
            cx.global_barrier()
            emit_ple_loads()
            def ple_load(gt):
                b = gt % 2
                hc = hcb[b]
                cx.dma("sp", lambda e: e.dma_start(out=hc[:, :], in_=hscr[gt * 128:(gt + 1) * 128, :]), reads=[("hscr", gt)], writes=[("hcb", b)])
                cx.dma("pool", lambda e: e.indirect_dma_start(out=y0[b][:, :], out_offset=None, in_=yscr[:, :], in_offset=bass.IndirectOffsetOnAxis(ap=posu[:, gt, 0:1], axis=0), bounds_check=NSLOT + 127, oob_is_err=False),
                       reads=["yscr", "posu"], writes=[("y0", b)])
                cx.dma("pool", lambda e: e.indirect_dma_start(out=y1[b][:, :], out_offset=None, in_=yscr[:, :], in_offset=bass.IndirectOffsetOnAxis(ap=posu[:, gt, 1:2], axis=0), bounds_check=NSLOT + 127, oob_is_err=False),
                       reads=["yscr", "posu"], writes=[("y1", b)])
            hu2b = [hu2, ring[:, 2, 0:D]]
            hT2b = [hT2, ring[:, 2, D:2 * D].rearrange("p (k t) -> p k t", k=KT)]
            sq2b = [sq2, ring[:, 2, 2 * D:3 * D]]

            def ple_front(gt):
                b = gt % 2
                ss2 = ss2a[:, b, :]
                hc = hcb[b]
                cx.op("dve", lambda e: e.scalar_tensor_tensor(out=hc[:, :], in0=y0[b][:, :], scalar=eww[:, gt, 0:1], in1=hc[:, :], op0=ALU.mult, op1=ALU.add),
                      reads=[("y0", b), "eww", ("hcb", b)], writes=[("hcb", b)], cost=2.2)
                cx.op("dve", lambda e: e.scalar_tensor_tensor(out=hc[:, :], in0=y1[b][:, :], scalar=eww[:, gt, 1:2], in1=hc[:, :], op0=ALU.mult, op1=ALU.add),
                      reads=[("y1", b), "eww", ("hcb", b)], writes=[("hcb", b)], cost=2.2)
                cx.op("act", lambda e: e.activation(out=sq2b[b][:, :], in_=hc[:, :], func=AF.Square, accum_out=ss2[:, 0:1]), reads=[("hcb", b), ("sq2", b), ("ss2", b)], writes=[("sq2", b), ("ss2", b)])
                cx.op("act", lambda e: e.activation(out=ss2[:, 1:2], in_=ss2[:, 0:1], func=AF.Sqrt, bias=eps_c[:], scale=1.0 / D), reads=[("ss2", b), "eps"], writes=[("ss2", b)])
                cx.op("dve", lambda e: e.reciprocal(out=ss2[:, 1:2], in_=ss2[:, 1:2]), reads=[("ss2", b)], writes=[("ss2", b)])
                cx.op("act", lambda e: e.activation(out=hu2b[b][:, :], in_=hc[:, :], func=AF.Copy, scale=ss2[:, 1:2]), reads=[("hcb", b), ("ss2", b), ("hu2", b)], writes=[("hu2", b)])
                for q in range(2):
                    pi = nextps()
                    pv = psb[pi][:, :].bitcast(BF16)
                    cx.mm([lambda e, j=j, pv=pv, q=q: e.transpose(out=pv[:, j * 128:(j + 1) * 128], in_=hu2b[b][:, (q * 8 + j) * 128:(q * 8 + j + 1) * 128], identity=ident_b[:]) for j in range(8)],
                          reads=[("hu2", b), "ident_b"], writes=[("ps", pi)])
                    cx.op("dve", lambda e, pv=pv, q=q: e.tensor_tensor(
                        out=hT2b[b][:, q * 8:(q + 1) * 8, :], in0=pv[:, :].rearrange("p (j t) -> p j t", j=8),
                        in1=colv2_t[:, q * 8:(q + 1) * 8].rearrange("p (j o) -> p j o", o=1).to_broadcast([128, 8, 128]), op=ALU.mult),
                        reads=[("ps", pi), "colv2", ("hT2", b)], writes=[("hT2", b)], cost=1.2)

            def ple_mid_back(gt):
                b = gt % 2
                ss2 = ss2a[:, b, :]
                hc = hcb[b]
                M = 128 if gt < 8 else NSC
                row0 = gt * 128 if gt < 8 else NPC
                for n in range(4):
                    pg_ = nextps(); pp_ = nextps()
                    cx.mm([lambda e, k=k, pg_=pg_, n=n: e.matmul(psb[pg_][0:M, :], lhsT=hT2b[b][:, k, 0:M], rhs=wpg_t[:, k, n * 512:(n + 1) * 512], start=(k == 0), stop=(k == KT - 1)) for k in range(KT)],
                          reads=[("hT2", b), ("wpgq", n)], writes=[("ps", pg_)])
                    cx.mm([lambda e, k=k, pp_=pp_, n=n: e.matmul(psb[pp_][0:M, :], lhsT=pTb[:, k, row0:row0 + M], rhs=wpp_t[:, k, n * 512:(n + 1) * 512], start=(k == 0), stop=(k == 1)) for k in range(2)],
                          reads=["pTb", "wpp"], writes=[("ps", pp_)])
                    cx.op("act", lambda e, pg_=pg_: e.activation(out=sgt[0][0:M, :], in_=psb[pg_][0:M, :], func=AF.Sigmoid), reads=[("ps", pg_)], writes=[("sgt", 0)])
                    cx.op("dve", lambda e, pp_=pp_: e.tensor_tensor(out=sgt[1][0:M, :], in0=psb[pp_][0:M, :], in1=sgt[0][0:M, :], op=ALU.mult), reads=[("ps", pp_), ("sgt", 0)], writes=[("sgt", 1)])
                    cx.op("dve", lambda e, n=n: e.tensor_tensor(out=hc[0:M, n * 512:(n + 1) * 512], in0=hc[0:M, n * 512:(n + 1) * 512], in1=sgt[1][0:M, :], op=ALU.add),
                          reads=[("sgt", 1), ("hcb", b)], writes=[("hcb", b)])
                cx.op("act", lambda e: e.activation(out=sq2b[b][0:M, :], in_=hc[0:M, :], func=AF.Square, accum_out=ss2[0:M, 2:3]), reads=[("hcb", b), ("sq2", b), ("ss2", b)], writes=[("sq2", b), ("ss2", b)])
                cx.op("act", lambda e: e.activation(out=ss2[0:M, 3:4], in_=ss2[0:M, 2:3], func=AF.Sqrt, bias=eps_c[0:M, :], scale=1.0 / D), reads=[("ss2", b), "eps"], writes=[("ss2", b)])
                cx.op("dve", lambda e: e.reciprocal(out=ss2[0:M, 3:4], in_=ss2[0:M, 3:4]), reads=[("ss2", b)], writes=[("ss2", b)])
                yo = yb[b]
                cx.op("dve", lambda e: e.scalar_tensor_tensor(out=yo[0:M, :], in0=hc[0:M, :], scalar=ss2[0:M, 3:4], in1=gfin_bc[0:M, :], op0=ALU.mult, op1=ALU.mult),
                      reads=[("hcb", b), ("ss2", b), "gfin_bc", ("yb", b)], writes=[("yb", b)], cost=2.2)
                cx.dma("sp", lambda e: e.dma_start(out=y[row0:row0 + M, :], in_=yo[0:M, :]), reads=[("yb", b)], writes=[("yout", gt)])

            ple_load(0)
            ple_load(1)
            ple_front(0)
            for gt in range(9):
                if gt + 1 < 9:
                    ple_front(gt + 1)
                ple_mid_back(gt)
                if gt + 2 < 9:
                    ple_load(gt + 2)
            cx.final_wait()
    return nc


_CACHE = {}


def _cols(v):
    return np.ascontiguousarray(np.asarray(v, np.float32).reshape(KT, 128).T)


def kernel(x_prompt, x_sample, state_conv, p_prompt, p_sample, norm_mix, w_in, ln_v_g, ln_v_b,
           w_spatial, b_spatial, w_proj_a, conv_w, conv_b, ln_c_g, ln_c_b, w_proj_b, w_out,
           norm_ffn, w_router_group, b_router_group, w_router_expert, b_router_expert,
           w_exp_gate, w_exp_up, w_exp_down, norm_ple, w_ple_gate, w_ple_proj, final_norm):
    f = lambda a: np.ascontiguousarray(np.asarray(a, dtype=np.float32))
    x_prompt = f(x_prompt); x_sample = f(x_sample); state_conv = f(state_conv)
    p_prompt = f(p_prompt); p_sample = f(p_sample)
    if "nc" not in _CACHE:
        _CACHE["nc"] = build_program()
    nc = _CACHE["nc"]

    ws = f(w_spatial)[0]
    wsd = np.repeat(ws[:, 0, 0], 256)
    bs0 = np.repeat(f(b_spatial)[0][:, 0], 256)
    colv = np.concatenate([_cols(f(norm_mix)[0]), _cols(f(ln_v_g)[0]), _cols(f(ln_v_b)[0]), _cols(f(conv_b)[0]),
                           _cols(f(ln_c_g)[0]), _cols(f(ln_c_b)[0]), _cols(wsd), _cols(bs0), _cols(f(norm_ffn)[0])], axis=1)
    colv2 = _cols(f(norm_ple)[0])
    cwh = np.ascontiguousarray(f(conv_w)[0].T.reshape(KT, 128, 31).transpose(1, 0, 2).reshape(128, KT * 31))
    wsT = np.ascontiguousarray(ws.transpose(2, 0, 1).reshape(128, 8 * 128))
    bsr = f(b_spatial)[0].reshape(1, 8 * 128)
    wr = np.ascontiguousarray(np.concatenate([f(w_router_group)[0], f(w_router_expert)[0]], axis=1))
    brr = np.concatenate([f(b_router_group)[0], f(b_router_expert)[0]]).reshape(1, 36)
    ident = np.eye(128, dtype=np.float32)
    ii = np.arange(128)
    maskT = (ii[:, None] <= ii[None, :]).astype(np.float32)
    triU = (ii[:, None] < ii[None, :]).astype(np.float32)
    base_e = np.tile((np.arange(32, dtype=np.float32) * CAP)[None, :], (128, 1))
    trash = (NSLOT + ii).astype(np.float32)[:, None]
    tokvalid = np.ones((128, 9), np.float32)
    tokvalid[NSC:, 8] = 0.0
    cst = np.ascontiguousarray(np.concatenate([ident, maskT, triU, base_e, trash, tokvalid], axis=1))

    shared = dict(
        w_in=f(w_in)[0], w_pa=f(w_proj_a)[0], w_pb=f(w_proj_b)[0], w_out=f(w_out)[0], w_pg=f(w_ple_gate)[0],
        w_pp=f(w_ple_proj)[0], w_eg=f(w_exp_gate)[0], w_eu=f(w_exp_up)[0], w_ed=f(w_exp_down)[0],
        wr=wr, br=brr, colv=np.ascontiguousarray(colv), colv2=colv2, cw=cwh, wsT=wsT, bs=bsr,
        gfin=f(final_norm).reshape(1, D), cst=cst)
    in_maps = []
    for c in range(NCORES):
        b, half = c // 2, c % 2
        xp = x_prompt[b, half * NPC:(half + 1) * NPC]
        xs = x_sample[c * NSC:(c + 1) * NSC, 0]
        xt = np.concatenate([xp, xs], axis=0)
        if half == 1:
            xhal = x_prompt[b, NPC - 32:NPC]
        else:
            xhal = np.zeros((32, D), np.float32)
        pt = np.concatenate([p_prompt[0, b, half * NPC:(half + 1) * NPC], p_sample[0, c * NSC:(c + 1) * NSC, 0]], axis=0)
        stc = state_conv[0, c * NSC:(c + 1) * NSC]
        m = dict(shared)
        m.update(xT=np.ascontiguousarray(xt.T), xh=np.ascontiguousarray(xhal.T), xtok=np.ascontiguousarray(xt),
                 pT=np.ascontiguousarray(pt.T), stT=np.ascontiguousarray(stc.transpose(2, 0, 1).reshape(D, NSC * 30)),
                 st=np.ascontiguousarray(stc.reshape(NSC, 30 * D)))
        in_maps.append(m)
    res = run_bass_kernel_spmd(nc, in_maps, core_ids=list(range(NCORES)))
    rs = res.results
    _CACHE["last"] = rs
    y_prompt = np.zeros((4, 2048, D), np.float32)
    y_sample = np.zeros((128, 1, D), np.float32)
    ncp_o = np.zeros((1, 4, 30, D), np.float32)
    ncs_o = np.zeros((1, 128, 30, D), np.float32)
    ncv_o = np.zeros((1, 128, 1, D), np.float32)
    for c in range(NCORES):
        b, half = c // 2, c % 2
        yy = np.asarray(rs[c]["y"])
        y_prompt[b, half * NPC:(half + 1) * NPC] = yy[:NPC]
        y_sample[c * NSC:(c + 1) * NSC, 0] = yy[NPC:]
        if half == 1:
            ncp_o[0, b] = np.asarray(rs[c]["ncp"])
        ncs_o[0, c * NSC:(c + 1) * NSC] = np.asarray(rs[c]["ncs"]).reshape(NSC, 30, D)
        ncv_o[0, c * NSC:(c + 1) * NSC, 0] = np.asarray(rs[c]["ncv"])
    return (y_prompt, y_sample, ncp_o, ncs_o, ncv_o)
```
